# Optimizing a Trainium2 kernel written in Bass

```python
import math
import jax
import jax.numpy as jnp
from jax import lax
import numpy as np

D_MODEL = 1024
BATCH = 8
SEQ = 2048
DEPTH = 2

MEM_LEN = 256
EPS = 1e-6

HY_C = D_MODEL // 2
HY_ORDER = 2
HY_SHORT = 3
FILT_BANDS = 16
FILT_EMB = 2 * FILT_BANDS + 1
FILT_HIDDEN = 64
DECAY_TARGET = 1e-2
FAST_DECAY_PCT = 0.3
SLOW_DECAY_PCT = 1.5
MOD_SHIFT = 0.05

AT_GROUPS = ((128, 1), (512, 4), (2048, 16))
AT_HEADS = 4
AT_HD = D_MODEL // 8
AT_W = len(AT_GROUPS) * AT_HEADS * AT_HD
AT_OUT = AT_HEADS * AT_HD
NUM_BUCKETS = 32
REL_MAX_DIST = 1024
NEG_INF = -1e30

XA_HEADS = 4
XA_HD = D_MODEL // 8
XA_W = XA_HEADS * XA_HD

D_FF = 2816
N_EXPERTS = 8
TOP_K = 2
MOE_BLOCK = 256

kernel_name = 'hybrid_hyena_dilated_moe_encoder'


def rmsnorm(x, g):
    xf = x.astype(jnp.float32)
    y = xf * lax.rsqrt(jnp.mean(xf * xf, axis=-1, keepdims=True) + EPS)
    return (y * g.astype(jnp.float32)).astype(x.dtype)


def rel_bucket(rel):
    half = NUM_BUCKETS // 2
    exact = half // 2
    n = np.abs(rel)
    large = exact + (np.log(np.maximum(n, 1) / exact) / np.log(REL_MAX_DIST / exact) * (half - exact)).astype(np.int32)
    large = np.minimum(large, half - 1)
    return (np.where(rel > 0, half, 0) + np.where(n < exact, n, large)).astype(np.int32)


def implicit_filters(L, w1, b1, w2, b2, w3, freq):
    t = jnp.linspace(0.0, 1.0, L, dtype=jnp.float32)[:, None]
    f = jnp.linspace(1e-4, FILT_BANDS - 1, FILT_BANDS, dtype=jnp.float32)[None]
    ang = (2.0 * math.pi / L) * jnp.arange(L, dtype=jnp.float32)[:, None] * f
    feats = jnp.concatenate([t, jnp.cos(ang), -jnp.sin(ang)], axis=-1)
    fr = freq.astype(jnp.float32)
    h = jnp.sin(fr * (feats @ w1.astype(jnp.float32) + b1.astype(jnp.float32)))
    h = jnp.sin(fr * (h @ w2.astype(jnp.float32) + b2.astype(jnp.float32)))
    h = (h @ w3.astype(jnp.float32)).reshape(L, HY_ORDER, 2, HY_C)
    deltas = jnp.abs(jnp.linspace(math.log(DECAY_TARGET) / SLOW_DECAY_PCT,
                                  math.log(DECAY_TARGET) / FAST_DECAY_PCT, HY_C, dtype=jnp.float32))
    h = h * (jnp.exp(-t[:, :, None, None] * deltas) + MOD_SHIFT)
    fwd = h[:, :, 0]
    bwd = h[1:, :, 1][::-1]
    filt = jnp.concatenate([fwd, jnp.zeros((1, HY_ORDER, HY_C), jnp.float32), bwd], axis=0)
    filt = filt / (jnp.sum(jnp.abs(filt), axis=0, keepdims=True) + 1e-6)
    return jnp.fft.rfft(filt, axis=0)


def fftconv(u, filt_f, skip):
    L = u.shape[1]
    uf = jnp.fft.rfft(u.astype(jnp.float32), n=2 * L, axis=1)
    y = jnp.fft.irfft(uf * filt_f[None], n=2 * L, axis=1)[:, :L]
    return y + u.astype(jnp.float32) * skip.astype(jnp.float32)


def hyena(u, conv_w, conv_b, filt_f, skip):
    pad = HY_SHORT // 2
    up = jnp.pad(u, ((0, 0), (pad, pad), (0, 0)))
    S = u.shape[1]
    uc = conv_b.astype(jnp.float32) + sum(up[:, j:j + S].astype(jnp.float32) * conv_w[j].astype(jnp.float32)
                                          for j in range(HY_SHORT))
    chunks = jnp.split(uc, HY_ORDER + 1, axis=-1)
    z = chunks[0]
    for o in range(HY_ORDER):
        z = chunks[o + 1] * fftconv(z, filt_f[:, o], skip[o])
    return z


def head_rms(t, g):
    tf = t.astype(jnp.float32)
    return tf * lax.rsqrt(jnp.mean(tf * tf, axis=-1, keepdims=True) + EPS) * g.astype(jnp.float32)


def dilated_group(q, k, v, window, dil, bias_table):
    B, S, H, HD = q.shape
    band = window // (2 * dil)
    L = S // dil
    nb = -(-L // band)
    Lp = nb * band

    def to_sub(t):
        return t.reshape(B, L, dil, H, HD).transpose(0, 2, 3, 1, 4)

    def key_blocks(t):
        tp = jnp.pad(to_sub(t), ((0, 0), (0, 0), (0, 0), (band, Lp - L + band), (0, 0)))
        tp = tp.reshape(B, dil, H, nb + 2, band, HD)
        return jnp.concatenate([tp[:, :, :, :-2], tp[:, :, :, 1:-1], tp[:, :, :, 2:]], axis=4)

    qb = jnp.pad(to_sub(q), ((0, 0), (0, 0), (0, 0), (0, Lp - L), (0, 0))).reshape(B, dil, H, nb, band, HD)
    kb = key_blocks(k)
    vb = key_blocks(v)
    rel = np.arange(3 * band)[None, :] - band - np.arange(band)[:, None]
    key_idx = np.arange(nb)[:, None] * band + np.arange(3 * band)[None, :] - band
    mask = (np.abs(rel) <= band)[None] & ((key_idx >= 0) & (key_idx < L))[:, None, :]
    bias = jnp.moveaxis(bias_table[rel_bucket(rel * dil)], -1, 0).astype(jnp.float32)
    s = jnp.einsum('brhnqd,brhnkd->brhnqk', qb, kb, preferred_element_type=jnp.float32) * (HD ** -0.5)
    s = jnp.where(mask, s + bias[None, None, :, None], NEG_INF)
    m = jnp.max(s, axis=-1, keepdims=True)
    p = jnp.exp(s - m)
    z = jnp.sum(p, axis=-1, keepdims=True)
    o = jnp.einsum('brhnqk,brhnkd->brhnqd', p, vb.astype(jnp.float32)) / z
    lse = (m + jnp.log(z))[..., 0]
    o = o.reshape(B, dil, H, Lp, HD)[:, :, :, :L].transpose(0, 3, 1, 2, 4).reshape(B, S, H, HD)
    lse = lse.reshape(B, dil, H, Lp)[..., :L].transpose(0, 3, 1, 2).reshape(B, S, H)
    return o, lse


def dilated_attention(qkv, q_gain, k_gain, rel_bias):
    B, S, _ = qkv.shape
    t = qkv.reshape(B, S, 3, len(AT_GROUPS), AT_HEADS, AT_HD)
    q = head_rms(t[:, :, 0], q_gain).astype(qkv.dtype)
    k = head_rms(t[:, :, 1], k_gain).astype(qkv.dtype)
    v = t[:, :, 2]
    outs, lses = [], []
    for g, (window, dil) in enumerate(AT_GROUPS):
        o, lse = dilated_group(q[:, :, g], k[:, :, g], v[:, :, g], window, dil,
                               rel_bias[:, g * AT_HEADS:(g + 1) * AT_HEADS])
        outs.append(o)
        lses.append(lse)
    w = jax.nn.softmax(jnp.stack(lses), axis=0)
    o = jnp.sum(w[..., None] * jnp.stack(outs), axis=0)
    return o.reshape(B, S, AT_OUT)


def cross_attention(xq, m, w_kv, q_gain, k_gain):
    B, S, _ = xq.shape
    M = m.shape[1]
    q = head_rms(xq.reshape(B, S, XA_HEADS, XA_HD), q_gain)
    kv = (m @ w_kv).reshape(B, M, 2, XA_HEADS, XA_HD)
    k = head_rms(kv[:, :, 0], k_gain)
    v = kv[:, :, 1].astype(jnp.float32)
    s = jnp.einsum('bshd,bmhd->bhsm', q, k) * (XA_HD ** -0.5)
    p = jax.nn.softmax(s, axis=-1)
    return jnp.einsum('bhsm,bmhd->bshd', p, v).reshape(B, S, XA_W)


def swiglu(h, wg, wu, wd):
    return (jax.nn.silu(h @ wg) * (h @ wu)) @ wd


def moe_swiglu(h, router, wg, wu, wd):
    B, S, D = h.shape
    T = B * S
    A = T * TOP_K
    hf = h.reshape(T, D)
    logits = jnp.dot(hf, router, preferred_element_type=jnp.float32)
    top_v, top_i = lax.top_k(logits, TOP_K)
    gates = jax.nn.softmax(top_v, axis=-1).reshape(A)
    expert = top_i.reshape(A)
    token = jnp.repeat(jnp.arange(T, dtype=jnp.int32), TOP_K)
    order = jnp.argsort(expert)
    e_s, tok_s, g_s = expert[order], token[order], gates[order]
    counts = jnp.bincount(expert, length=N_EXPERTS)
    padded = (counts + MOE_BLOCK - 1) // MOE_BLOCK * MOE_BLOCK
    start = jnp.cumsum(counts) - counts
    pend = jnp.cumsum(padded)
    pstart = pend - padded
    dest = pstart[e_s] + jnp.arange(A, dtype=jnp.int32) - start[e_s]
    P = A + N_EXPERTS * MOE_BLOCK
    nblk = P // MOE_BLOCK
    buf_tok = jnp.zeros((P,), jnp.int32).at[dest].set(tok_s)
    buf_gate = jnp.zeros((P,), jnp.float32).at[dest].set(g_s)
    blk_expert = jnp.minimum(jnp.searchsorted(pend, jnp.arange(nblk, dtype=jnp.int32) * MOE_BLOCK, side='right'),
                             N_EXPERTS - 1)
    xb = hf[buf_tok].reshape(nblk, MOE_BLOCK, D)

    def expert_block(args):
        xe, e = args
        return (jax.nn.silu(xe @ wg[e]) * (xe @ wu[e])) @ wd[e]

    yb = lax.map(expert_block, (xb, blk_expert)).reshape(P, D)
    out = jnp.zeros((T, D), jnp.float32).at[buf_tok].add(yb.astype(jnp.float32) * buf_gate[:, None])
    return out.reshape(B, S, D).astype(h.dtype)


def setup_inputs(seed: int = 0) -> dict:
    key = jax.random.key(seed)
    ks = iter(jax.random.split(key, 32))
    NHY = (DEPTH + 1) // 2
    NAT = DEPTH // 2

    def nrm(shape, scale):
        return jax.random.normal(next(ks), shape, jnp.float32) * scale

    def gain(shape):
        return 1.0 + nrm(shape, 0.02)

    return {
        'x': nrm((BATCH, SEQ, D_MODEL), 1.0),
        'mem': nrm((BATCH, MEM_LEN, D_MODEL), 1.0),
        'rel_bias': nrm((NUM_BUCKETS, len(AT_GROUPS) * AT_HEADS), 0.5),
        'norm_mix': gain((DEPTH, D_MODEL)),
        'norm_mem': gain((DEPTH, D_MODEL)),
        'norm_ffn': gain((DEPTH, D_MODEL)),
        'w_mem_kv': nrm((DEPTH, D_MODEL, 2 * XA_W), D_MODEL ** -0.5),
        'xq_norm': gain((DEPTH, XA_HD)),
        'xk_norm': gain((DEPTH, XA_HD)),
        'w_out': nrm((DEPTH, D_MODEL, D_MODEL), D_MODEL ** -0.5),
        'hy_w_in': nrm((NHY, D_MODEL, (HY_ORDER + 1) * HY_C + XA_W), D_MODEL ** -0.5),
        'hy_conv_w': nrm((NHY, HY_SHORT, (HY_ORDER + 1) * HY_C), HY_SHORT ** -0.5),
        'hy_conv_b': nrm((NHY, (HY_ORDER + 1) * HY_C), 0.02),
        'hy_filt_w1': nrm((NHY, FILT_EMB, FILT_HIDDEN), FILT_EMB ** -0.5),
        'hy_filt_b1': nrm((NHY, FILT_HIDDEN), 0.1),
        'hy_filt_w2': nrm((NHY, FILT_HIDDEN, FILT_HIDDEN), FILT_HIDDEN ** -0.5),
        'hy_filt_b2': nrm((NHY, FILT_HIDDEN), 0.1),
        'hy_filt_w3': nrm((NHY, FILT_HIDDEN, HY_ORDER * 2 * HY_C), FILT_HIDDEN ** -0.5),
        'hy_sin_freq': 1.0 + nrm((NHY, FILT_HIDDEN), 0.1),
        'hy_skip': nrm((NHY, HY_ORDER, HY_C), 1.0),
        'at_w_in': nrm((NAT, D_MODEL, 3 * AT_W + XA_W), D_MODEL ** -0.5),
        'at_q_norm': gain((NAT, AT_HD)),
        'at_k_norm': gain((NAT, AT_HD)),
        'ffn_w_gate': nrm((NHY, D_MODEL, D_FF), D_MODEL ** -0.5),
        'ffn_w_up': nrm((NHY, D_MODEL, D_FF), D_MODEL ** -0.5),
        'ffn_w_down': nrm((NHY, D_FF, D_MODEL), D_FF ** -0.5),
        'moe_router': nrm((NAT, D_MODEL, N_EXPERTS), D_MODEL ** -0.5),
        'moe_w_gate': nrm((NAT, N_EXPERTS, D_MODEL, D_FF), D_MODEL ** -0.5),
        'moe_w_up': nrm((NAT, N_EXPERTS, D_MODEL, D_FF), D_MODEL ** -0.5),
        'moe_w_down': nrm((NAT, N_EXPERTS, D_FF, D_MODEL), D_FF ** -0.5),
    }


def reference(x, mem, rel_bias, norm_mix, norm_mem, norm_ffn, w_mem_kv, xq_norm, xk_norm, w_out,
              hy_w_in, hy_conv_w, hy_conv_b, hy_filt_w1, hy_filt_b1, hy_filt_w2, hy_filt_b2, hy_filt_w3,
              hy_sin_freq, hy_skip, at_w_in, at_q_norm, at_k_norm, ffn_w_gate, ffn_w_up, ffn_w_down,
              moe_router, moe_w_gate, moe_w_up, moe_w_down):
    S = x.shape[1]
    for i in range(DEPTH):
        j = i // 2
        h = rmsnorm(x, norm_mix[i])
        m = rmsnorm(mem, norm_mem[i])
        if i % 2 == 0:
            z = h @ hy_w_in[j]
            filt_f = implicit_filters(S, hy_filt_w1[j], hy_filt_b1[j], hy_filt_w2[j], hy_filt_b2[j],
                                      hy_filt_w3[j], hy_sin_freq[j])
            self_out = hyena(z[..., :(HY_ORDER + 1) * HY_C], hy_conv_w[j], hy_conv_b[j], filt_f, hy_skip[j])
            xq = z[..., (HY_ORDER + 1) * HY_C:]
        else:
            z = h @ at_w_in[j]
            self_out = dilated_attention(z[..., :3 * AT_W], at_q_norm[j], at_k_norm[j], rel_bias)
            xq = z[..., 3 * AT_W:]
        cross = cross_attention(xq, m, w_mem_kv[i], xq_norm[i], xk_norm[i])
        mixed = jnp.concatenate([self_out, cross], axis=-1).astype(x.dtype)
        x = x + mixed @ w_out[i]
        hf = rmsnorm(x, norm_ffn[i])
        if i % 2 == 0:
            x = x + swiglu(hf, ffn_w_gate[j], ffn_w_up[j], ffn_w_down[j])
        else:
            x = x + moe_swiglu(hf, moe_router[j], moe_w_gate[j], moe_w_up[j], moe_w_down[j])
    return x
```

```python
import contextlib
import math
import os
HY_STOP = int(os.environ.get('HY_STOP', '9'))
HY_F = int(os.environ.get('HY_F', '9'))
import numpy as np
import concourse.bass as bass
import concourse.mybir as mybir
from concourse.bass_utils import run_bass_kernel_spmd

F32 = mybir.dt.float32
BF16 = mybir.dt.bfloat16
AF = mybir.ActivationFunctionType
ALU = mybir.AluOpType
AX = mybir.AxisListType

TOK = 2048
D = 1024
DC = 8
NT = 16
MEM = 256
DFF = 2816
NFF = 22
NE = 8
EPS = 1e-6
ADT = BF16
SAME_ENGINE_SYNC = True
ARENA_BYTES = 174 * 1024


class T:
    __slots__ = ("w", "r")

    def __init__(self):
        self.w = None
        self.r = {}


def TL(n):
    return [T() for _ in range(n)]


class Sched:
    ENGS = ("pe", "act", "dve", "pool", "sp")
    NDMA = {"sp": 8, "pool": 4, "act": 4}

    def __init__(self, nc, stack):
        self.nc = nc
        self.streams = {e: [] for e in self.ENGS}
        self.cnt = {e: 0 for e in self.ENGS}
        self.sems = {}
        for e in ("pe", "act", "dve", "pool"):
            self.sems[e] = stack.enter_context(nc.semaphore("c_" + e))
        self.dma_i = {q: 0 for q in self.NDMA}
        for q, k in self.NDMA.items():
            for s in range(k):
                self.sems[("dma", q, s)] = stack.enter_context(nc.semaphore("d_%s_%d" % (q, s)))
        self.waited = {}
        self.final = {}

    def _collect(self, eng, reads, writes, skip_self):
        deps = {}

        def add(ev):
            if ev is None:
                return
            k, v = ev
            if deps.get(k, 0) < v:
                deps[k] = v
        for t in reads:
            add(t.w)
        for t in writes:
            add(t.w)
            for k, v in t.r.items():
                add((k, v))
        waits = []
        for k, v in deps.items():
            if k == eng and (skip_self or not SAME_ENGINE_SYNC):
                continue
            if self.waited.get((eng, k), 0) >= v:
                continue
            self.waited[(eng, k)] = v
            waits.append((k, v))
        return waits

    def _mark(self, ev, reads, writes):
        k, v = ev
        for t in reads:
            t.r[k] = v
        for t in writes:
            t.w = ev
            t.r = {}
        if self.final.get(k, 0) < v:
            self.final[k] = v

    def op(self, eng, fn, reads=(), writes=(), skip_self=None):
        if skip_self is None:
            skip_self = (eng == "pe")
        waits = self._collect(eng, reads, writes, skip_self)
        self.cnt[eng] += 1
        ev = (eng, self.cnt[eng])
        self.streams[eng].append((waits, fn, (eng, 1)))
        self._mark(ev, reads, writes)
        return ev

    def dma(self, q, out, in_, reads=(), writes=(), **kw):
        i = self.dma_i[q]
        K = self.NDMA[q]
        slot = i % K
        key = ("dma", q, slot)
        waits = self._collect(q, reads, writes, False)
        if i >= K:
            prev = 16 * (i // K)
            if self.waited.get((q, key), 0) < prev:
                self.waited[(q, key)] = prev
                waits.append((key, prev))
        self.dma_i[q] = i + 1
        ev = (key, 16 * (i // K + 1))

        def fn(e, out=out, in_=in_, kw=kw):
            return e.dma_start(out=out, in_=in_, **kw)
        self.streams[q].append((waits, fn, (key, 16)))
        self._mark(ev, reads, writes)
        return ev

    def barrier(self):
        for e in self.ENGS:
            waits = []
            for k, v in self.final.items():
                if k == e:
                    continue
                if self.waited.get((e, k), 0) >= v:
                    continue
                self.waited[(e, k)] = v
                waits.append((k, v))
            if waits:
                self.streams[e].append((waits, None, None))

    def emit(self):
        nc = self.nc
        self.barrier()
        sems = self.sems
        streams = self.streams

        def run(e, name):
            for waits, fn, inc in streams[name]:
                for k, v in waits:
                    e.wait_ge(sems[k], v)
                if fn is not None:
                    inst = fn(e)
                    inst.then_inc(sems[inc[0]], inc[1])

        with nc.Block() as block:
            @block.tensor
            def _(e):
                run(e, "pe")

            @block.scalar
            def _(e):
                run(e, "act")

            @block.vector
            def _(e):
                run(e, "dve")

            @block.gpsimd
            def _(e):
                run(e, "pool")

            @block.sync
            def _(e):
                run(e, "sp")


def dtsize(dt):
    return mybir.dt.size(dt)


class Ctx:
    def __init__(self, nc, stack):
        self.nc = nc
        self.S = Sched(nc, stack)
        self.arena = stack.enter_context(nc.sbuf_tensor("arena", [128, ARENA_BYTES // 4], F32))[:, :]
        self.off = 0
        self.banks = [stack.enter_context(nc.psum_tensor("bank%d" % i, [128, 512], F32))[:, :] for i in range(8)]
        self.bank_t = TL(8)
        self.bank_i = 0
        self.reserved = set()
        self.drams = {}

    def alloc(self, shape, dt=F32):
        per = int(np.prod(shape[1:])) * dtsize(dt)
        per = (per + 31) // 32 * 32
        assert self.off + per <= ARENA_BYTES, ("SBUF arena overflow", self.off, per)
        v = self.arena[0:shape[0], self.off // 4:(self.off + per) // 4]
        self.off += per
        self.peak = max(getattr(self, 'peak', 0), self.off)
        if dt != F32:
            v = v.bitcast(dt)
        n = int(np.prod(shape[1:]))
        v = v[:, 0:n]
        if len(shape) == 3:
            v = v.rearrange("p (a b) -> p a b", a=shape[1])
        elif len(shape) == 4:
            v = v.rearrange("p (a b c) -> p a b c", a=shape[1], b=shape[2])
        return v

    @contextlib.contextmanager
    def scope(self):
        save = self.off
        yield
        self.S.barrier()
        self.off = save

    def psum(self):
        while True:
            i = self.bank_i
            self.bank_i = (i + 1) % 8
            if i not in self.reserved:
                return self.banks[i], self.bank_t[i]

    def reserve(self):
        i = self.bank_i
        self.bank_i = (i + 1) % 8
        self.reserved.add(i)
        return i

    def release(self, i):
        self.reserved.discard(i)

    def mm(self, out, pairs, reads, writes, **kw):
        pairs = list(pairs)

        def fn(e, out=out, pairs=pairs, kw=kw):
            n = len(pairs)
            inst = None
            for i, (l, r) in enumerate(pairs):
                inst = e.matmul(out, l, r, start=(i == 0), stop=(i == n - 1), **kw)
            return inst
        return self.S.op("pe", fn, reads=reads, writes=writes)


def load_consts(C, W):
    S = C.S
    C.ident_f = C.alloc([128, 128], F32)
    C.ident_b = C.alloc([128, 128], BF16)
    C.ones_f = C.alloc([128, 128], F32)
    C.ones_b = C.alloc([128, 128], BF16)
    C.ct = T()
    S.dma("sp", C.ident_f, W["c_ident"], writes=[C.ct])
    S.op("dve", lambda e: e.tensor_copy(C.ident_b, C.ident_f), reads=[C.ct], writes=[C.ct])
    S.op("dve", lambda e: e.memset(C.ones_f, 1.0), writes=[C.ct])
    S.op("dve", lambda e: e.memset(C.ones_b, 1.0), writes=[C.ct])


def rstd_from_ss(C, out, ss, n, reads, writes, tmp):
    S = C.S
    S.op("act", lambda e: e.activation(tmp, ss, AF.Ln, bias=EPS, scale=1.0 / n), reads=list(reads), writes=writes)
    S.op("act", lambda e: e.activation(out, tmp, AF.Exp, scale=-0.5), reads=writes, writes=writes)


def norm_T(C, x_ap, ntok, g_ap, hT, hT_t, hook=None, copy_to=None, copy_t=None, hook_xn=False):
    S = C.S
    nt = ntok // 128
    with C.scope():
        g_sb = C.alloc([128, 8]); g_t = T()
        S.dma("sp", g_sb, g_ap, writes=[g_t])
        xt = [C.alloc([128, 1024]) for _ in range(2)]; xt_t = TL(2)
        xn = [C.alloc([128, 1024]) for _ in range(2)]; xn_t = TL(2)
        junk = C.alloc([128, 1024], BF16); junk_t = T()
        st = C.alloc([128, 3 * nt]); st_t = TL(nt)
        h32 = None
        if hook is not None:
            h32 = [C.alloc([128, 8, 128]) for _ in range(2)]; h32_t = TL(2)
        for t in range(nt):
            s = t % 2
            S.dma("sp", xt[s], x_ap[t * 128:(t + 1) * 128, :], writes=[xt_t[s]])
            if copy_to is not None:
                S.dma("sp", copy_to[t * 128:(t + 1) * 128, :], xt[s], reads=[xt_t[s]], writes=[copy_t[t]])
            ss = st[:, 3 * t:3 * t + 1]; ln = st[:, 3 * t + 1:3 * t + 2]; rs = st[:, 3 * t + 2:3 * t + 3]
            S.op("act", lambda e, s=s, ss=ss: e.activation(junk, xt[s], AF.Square, accum_out=ss),
                 reads=[xt_t[s]], writes=[junk_t, st_t[t]])
            rstd_from_ss(C, rs, ss, 1024.0, [st_t[t]], [st_t[t]], ln)
            S.op("dve", lambda e, s=s, rs=rs: e.tensor_scalar(xn[s], xt[s], rs, None, ALU.mult),
                 reads=[xt_t[s], st_t[t]], writes=[xn_t[s]])
            for half in range(2):
                ps, pt = C.psum()

                def tr(e, s=s, half=half, ps=ps):
                    inst = None
                    for j in range(4):
                        c = half * 4 + j
                        inst = e.transpose(ps[:, j * 128:(j + 1) * 128], xn[s][:, c * 128:(c + 1) * 128], C.ident_f)
                    return inst
                S.op("pe", tr, reads=[xn_t[s], C.ct], writes=[pt])
                gb = g_sb[:, half * 4:half * 4 + 4].unsqueeze(2).to_broadcast([128, 4, 128])
                psv = ps[:].rearrange("p (a b) -> p a b", a=4)
                if hook is None:
                    S.op("dve", lambda e, t=t, half=half, psv=psv, gb=gb: e.tensor_tensor(
                        hT[:, half * 4:half * 4 + 4, t * 128:(t + 1) * 128], psv, gb, ALU.mult),
                        reads=[pt, g_t], writes=[hT_t[t]])
                else:
                    S.op("dve", lambda e, s=s, half=half, psv=psv, gb=gb: e.tensor_tensor(
                        h32[s][:, half * 4:half * 4 + 4, :], psv, gb, ALU.mult),
                        reads=[pt, g_t], writes=[h32_t[s]])
                    if hT is not None:
                        S.op("act", lambda e, s=s, t=t, half=half: e.copy(
                            hT[:, half * 4:half * 4 + 4, t * 128:(t + 1) * 128], h32[s][:, half * 4:half * 4 + 4, :]),
                            reads=[h32_t[s]], writes=[hT_t[t]])
            if hook is not None:
                if hook_xn:
                    hook(t, h32[s], h32_t[s], xn[s], xn_t[s])
                else:
                    hook(t, h32[s], h32_t[s])


class WStream:
    def __init__(self, C, nstage=3, nslot=4, elems=2048):
        self.C = C
        self.stage = [C.alloc([128, elems], F32) for _ in range(nstage)]
        self.stage_t = TL(nstage)
        self.slot = [C.alloc([128, elems], BF16) for _ in range(nslot)]
        self.slot_t = TL(nslot)
        self.i = 0
        self.j = 0
        self.pending = []

    def issue(self, src_ap, shape, cast_eng):
        C = self.C; S = C.S
        si = self.i % len(self.stage); self.i += 1
        sj = self.j % len(self.slot); self.j += 1
        n = int(np.prod(shape[1:]))
        stg = self.stage[si][:, 0:n]
        dst = self.slot[sj][:, 0:n]
        if len(shape) == 3:
            stg3 = stg.rearrange("p (a b) -> p a b", a=shape[1])
            dst3 = dst.rearrange("p (a b) -> p a b", a=shape[1])
        else:
            stg3, dst3 = stg, dst
        S.dma("sp", stg3, src_ap, writes=[self.stage_t[si]])
        if cast_eng == "act":
            S.op("act", lambda e: e.copy(dst, stg), reads=[self.stage_t[si]], writes=[self.slot_t[sj]])
        elif cast_eng == "dve":
            S.op("dve", lambda e: e.tensor_copy(dst, stg), reads=[self.stage_t[si]], writes=[self.slot_t[sj]])
        else:
            S.op("pool", lambda e: e.tensor_copy(dst, stg), reads=[self.stage_t[si]], writes=[self.slot_t[sj]])
        return dst3, self.slot_t[sj]


def wgu_slab(Wt, e, s_):
    return Wt[e, s_]


def wd_slab(Wt, e, half, s4, nf):
    return Wt[e, half, s4][:, 0:nf, :]


def wview(w_ap, c0, nc_, n0, nn):
    return w_ap.rearrange("(c p) n -> p c n", p=128)[:, c0:c0 + nc_, n0:n0 + nn]


def stage_ffn(C, x_in, x_out, g_ap, Wg, Wu, Wd, router=None, sel=None):
    S = C.S
    E = Wg.shape[0]
    moe = router is not None
    with C.scope():
        hT = C.alloc([128, 8, TOK], ADT); hT_t = TL(NT)
        GT = None
        if moe:
            GT = C.alloc([8, TOK], F32); GT_t = TL(NT)
            r_sb = C.alloc([128, 8, 8], F32); r_t = T()
            S.dma("sp", r_sb, router.rearrange("(c p) e -> p c e", p=128), writes=[r_t])
            sel_sb = C.alloc([8, NE * 128], F32); sel_t = T()
            S.dma("sp", sel_sb, sel, writes=[sel_t])
            gsm = [C.alloc([128, 64], F32) for _ in range(2)]; gsm_t = TL(2)

            def hook(t, h32, h32_t):
                s = t % 2
                ps, pt = C.psum()
                C.mm(ps[:, 0:8], [(h32[:, c, :], r_sb[:, c, :]) for c in range(8)], [h32_t, r_t], [pt])
                g = gsm[s]
                lg = g[:, 0:8]; top = g[:, 8:16]; nv1 = g[:, 16:17]; msk = g[:, 24:32]; ex = g[:, 32:40]
                mex = g[:, 40:48]; den = g[:, 48:49]; rden = g[:, 49:50]; G = g[:, 56:64]
                S.op("dve", lambda e: e.tensor_copy(lg, ps[:, 0:8]), reads=[pt], writes=[gsm_t[s]])
                S.op("dve", lambda e: e.max(top, lg), reads=[gsm_t[s]], writes=[gsm_t[s]])
                S.op("dve", lambda e: e.tensor_scalar(nv1, top[:, 0:1], -1.0, None, ALU.mult), reads=[gsm_t[s]], writes=[gsm_t[s]])
                S.op("dve", lambda e: e.tensor_scalar(msk, lg, top[:, 1:2], None, ALU.is_ge), reads=[gsm_t[s]], writes=[gsm_t[s]])
                S.op("act", lambda e: e.activation(ex, lg, AF.Exp, bias=nv1, scale=1.0), reads=[gsm_t[s]], writes=[gsm_t[s]])
                S.op("dve", lambda e: e.tensor_tensor(mex, msk, ex, ALU.mult), reads=[gsm_t[s]], writes=[gsm_t[s]])
                S.op("dve", lambda e: e.reduce_sum(den, mex, AX.X), reads=[gsm_t[s]], writes=[gsm_t[s]])
                S.op("dve", lambda e: e.reciprocal(rden, den), reads=[gsm_t[s]], writes=[gsm_t[s]])
                S.op("dve", lambda e: e.tensor_scalar(G, mex, rden, None, ALU.mult), reads=[gsm_t[s]], writes=[gsm_t[s]])
                ps2, pt2 = C.psum()
                S.op("pe", lambda e: e.transpose(ps2[0:8, 0:128], G, C.ident_f), reads=[gsm_t[s], C.ct], writes=[pt2])
                S.op("dve", lambda e: e.tensor_copy(GT[:, t * 128:(t + 1) * 128], ps2[0:8, 0:128]), reads=[pt2], writes=[GT_t[t]])
            norm_T(C, x_in, TOK, g_ap, hT, hT_t, hook=hook)
        else:
            norm_T(C, x_in, TOK, g_ap, hT, hT_t)

        TG = 1024
        NTG = TOK // TG
        aT = C.alloc([128, NFF, TG], ADT); aT_t = TL(NFF)
        yacc = C.alloc([128, 8, 1024], F32); yacc_t = TL(16)
        sg = [C.alloc([128, 512], F32) for _ in range(2)]; sg_t = TL(2)
        grep = C.alloc([128, TG], ADT); grep_t = T()
        ws = WStream(C, nstage=3, nslot=4)
        xin_v = x_in.rearrange("(t p) d -> p t d", p=128)
        xout_v = x_out.rearrange("(t p) d -> p t d", p=128)
        k = 0
        for tg in range(NTG):
            ya = yacc; ya_t = yacc_t
            S.dma("sp", ya, xin_v[:, tg * 8:(tg + 1) * 8, :], writes=ya_t)
            for ex_i in range(E):
                if moe:
                    for blk in range(2):
                        tok = slice(tg * TG + blk * 512, tg * TG + (blk + 1) * 512)
                        ps, pt = C.psum()
                        C.mm(ps, [(sel_sb[:, ex_i * 128:(ex_i + 1) * 128], GT[:, tok])], [sel_t] + GT_t[tg * 8:tg * 8 + 8], [pt])
                        S.op("act", lambda e, ps=ps, blk=blk: e.copy(grep[:, blk * 512:(blk + 1) * 512], ps), reads=[pt], writes=[grep_t])
                for s in range(11):
                    wg_b, wg_t = ws.issue(wgu_slab(Wg, ex_i, s), [128, 8, 256], "pool")
                    wu_b, wu_t = ws.issue(wgu_slab(Wu, ex_i, s), [128, 8, 256], "pool")
                    for j in range(2):
                        ff = 2 * s + j
                        for blk in range(2):
                            tok = slice(tg * TG + blk * 512, tg * TG + (blk + 1) * 512)
                            lt = slice(blk * 512, (blk + 1) * 512)
                            hts = hT_t[tg * 8 + blk * 4:tg * 8 + blk * 4 + 4]
                            pg, pgt = C.psum()
                            C.mm(pg, [(wg_b[:, c, j * 128:(j + 1) * 128], hT[:, c, tok]) for c in range(8)], [wg_t] + hts, [pgt])
                            pu, put = C.psum()
                            C.mm(pu, [(wu_b[:, c, j * 128:(j + 1) * 128], hT[:, c, tok]) for c in range(8)], [wu_t] + hts, [put])
                            sgi = k % 2; k += 1
                            S.op("act", lambda e, sgi=sgi, pg=pg: e.activation(sg[sgi], pg, AF.Silu), reads=[pgt], writes=[sg_t[sgi]])
                            if moe:
                                S.op("dve", lambda e, sgi=sgi, pu=pu: e.tensor_tensor(sg[sgi], sg[sgi], pu, ALU.mult),
                                     reads=[put, sg_t[sgi]], writes=[sg_t[sgi]])
                                S.op("pool", lambda e, sgi=sgi, ff=ff, lt=lt: e.tensor_tensor(aT[:, ff, lt], sg[sgi], grep[:, lt], ALU.mult),
                                     reads=[sg_t[sgi], grep_t], writes=[aT_t[ff]])
                            else:
                                S.op("dve", lambda e, sgi=sgi, pu=pu, ff=ff, lt=lt: e.tensor_tensor(aT[:, ff, lt], sg[sgi], pu, ALU.mult),
                                     reads=[put, sg_t[sgi]], writes=[aT_t[ff]])
                for half in range(2):
                    for s4 in range(6):
                        nf = 4 if s4 < 5 else 2
                        wd_b, wd_t = ws.issue(wd_slab(Wd, ex_i, half, s4, nf), [128, nf, 512], "act")

                        def dn(e, s4=s4, nf=nf, wd_b=wd_b):
                            inst = None
                            for j in range(nf):
                                ff = 4 * s4 + j
                                for tt in range(8):
                                    inst = e.matmul(C.banks[tt], aT[:, ff, tt * 128:(tt + 1) * 128], wd_b[:, j, :],
                                                    start=(ff == 0), stop=(ff == NFF - 1))
                            return inst
                        S.op("pe", dn, reads=[wd_t] + aT_t[4 * s4:4 * s4 + nf], writes=C.bank_t)
                    for tt in range(8):
                        b = tt * 2 + half
                        S.op("dve", lambda e, tt=tt, half=half: e.tensor_tensor(
                            ya[:, tt, half * 512:(half + 1) * 512], ya[:, tt, half * 512:(half + 1) * 512], C.banks[tt], ALU.add),
                            reads=[C.bank_t[tt], ya_t[b]], writes=[ya_t[b]])
            S.dma("sp", xout_v[:, tg * 8:(tg + 1) * 8, :], ya, reads=ya_t)


CAP = 768
NSL = CAP // 128
NBH = CAP // 2


def stage_moe_sparse(C, x_in, x_out, g_ap, grow_ap, Wg, Wu, Wd, router, W):
    S = C.S
    with C.scope():
        htm = C.alloc([128, NT, 1024], ADT); htm_t = TL(NT)
        pos_sb = C.alloc([128, NT, 8], F32); msk_sb = C.alloc([128, NT, 8], F32); G_sb = C.alloc([128, NT, 8], F32); rt_t = TL(NT)
        msum = C.alloc([128, 8], F32); msum_t = T()
        S.op("pool", lambda e: e.memset(msum, 0.0), writes=[msum_t])
        iota_row = C.alloc([128, CAP], F32); slotcol = C.alloc([128, NSL], F32); tri = C.alloc([128, 128], F32); cst_t = T()
        S.dma("sp", iota_row, W["c_iota"].partition_broadcast(128), writes=[cst_t])
        S.dma("sp", slotcol, W["c_slotcol"], writes=[cst_t])
        S.dma("sp", tri, W["c_tri"], writes=[cst_t])
        xo_t = TL(NT)
        with C.scope():
            r_sb = C.alloc([128, 8, 8], F32); r_t = T()
            S.dma("sp", r_sb, router.rearrange("(c p) e -> p c e", p=128), writes=[r_t])
            grow = C.alloc([128, 1024], F32); grow_t = T()
            S.dma("sp", grow, grow_ap.partition_broadcast(128), writes=[grow_t])
            gsm = [C.alloc([128, 64], F32) for _ in range(2)]; gsm_t = TL(2)

            def hook(t, h32, h32_t, xn, xn_t):
                s = t % 2
                S.op("pool", lambda e: e.tensor_tensor(htm[:, t, :], xn, grow, ALU.mult), reads=[xn_t, grow_t], writes=[htm_t[t]])
                ps, pt = C.psum()
                C.mm(ps[:, 0:8], [(h32[:, c, :], r_sb[:, c, :]) for c in range(8)], [h32_t, r_t], [pt])
                g = gsm[s]
                lg = g[:, 0:8]; top = g[:, 8:16]; nv1 = g[:, 16:17]; ex = g[:, 32:40]
                mex = g[:, 40:48]; den = g[:, 48:49]; rden = g[:, 49:50]
                msk = msk_sb[:, t, :]
                S.op("dve", lambda e: e.tensor_copy(lg, ps[:, 0:8]), reads=[pt], writes=[gsm_t[s]])
                S.op("dve", lambda e: e.max(top, lg), reads=[gsm_t[s]], writes=[gsm_t[s]])
                S.op("dve", lambda e: e.tensor_scalar(nv1, top[:, 0:1], -1.0, None, ALU.mult), reads=[gsm_t[s]], writes=[gsm_t[s]])
                S.op("dve", lambda e: e.tensor_scalar(msk, lg, top[:, 1:2], None, ALU.is_ge), reads=[gsm_t[s]], writes=[rt_t[t]])
                S.op("act", lambda e: e.activation(ex, lg, AF.Exp, bias=nv1, scale=1.0), reads=[gsm_t[s]], writes=[gsm_t[s]])
                S.op("dve", lambda e: e.tensor_tensor(mex, msk, ex, ALU.mult), reads=[gsm_t[s], rt_t[t]], writes=[gsm_t[s]])
                S.op("dve", lambda e: e.reduce_sum(den, mex, AX.X), reads=[gsm_t[s]], writes=[gsm_t[s]])
                S.op("dve", lambda e: e.reciprocal(rden, den), reads=[gsm_t[s]], writes=[gsm_t[s]])
                S.op("dve", lambda e: e.tensor_scalar(G_sb[:, t, :], mex, rden, None, ALU.mult), reads=[gsm_t[s]], writes=[rt_t[t]])
                ps2, pt2 = C.psum()
                C.mm(ps2[:, 0:8], [(tri, msk), (C.ones_f, msum)], [rt_t[t], msum_t, cst_t, C.ct], [pt2])
                S.op("dve", lambda e: e.tensor_copy(pos_sb[:, t, :], ps2[:, 0:8]), reads=[pt2], writes=[rt_t[t]])
                S.op("dve", lambda e: e.tensor_tensor(msum, msum, msk, ALU.add), reads=[rt_t[t], msum_t], writes=[msum_t])
            norm_T(C, x_in, TOK, g_ap, None, None, hook=hook, copy_to=x_out, copy_t=xo_t, hook_xn=True)

        ws = WStream(C, nstage=3, nslot=3)
        Sel = C.alloc([128, NT, NBH], ADT); Sel_t = TL(NT)
        hgT = C.alloc([128, 8, CAP], ADT); hg_t = TL(2)
        aT = C.alloc([128, NFF, CAP], ADT); aT_t = TL(NFF)
        ye = C.alloc([128, NSL, 1024], ADT); ye_t = TL(NSL)
        sg = [C.alloc([128, NBH], F32) for _ in range(2)]; sg_t = TL(2)
        dgp = [C.alloc([128, 128], F32) for _ in range(2)]; dgg = [C.alloc([128, 128], F32) for _ in range(2)]; dg_t = TL(2)
        grep = [C.alloc([128, 256], F32) for _ in range(2)]; grep_t = TL(2)
        SelGT = [C.alloc([128, NSL, 256], ADT) for _ in range(2)]; SelGT_t = TL(2)
        ystg = [C.alloc([128, 2, 1024], F32) for _ in range(2)]; ystg_t = TL(2)
        xout_v = x_out.rearrange("(t p) d -> p t d", p=128)
        ksg = 0; kd = 0; ktp = 0
        pre_issued = []
        for ex_i in range(NE):
            for nb in range(2):
                for t in range(NT):
                    S.op("dve", lambda e, t=t, nb=nb, ex_i=ex_i: e.tensor_scalar(
                        Sel[:, t, :], iota_row[:, nb * NBH:(nb + 1) * NBH], pos_sb[:, t, ex_i:ex_i + 1], msk_sb[:, t, ex_i:ex_i + 1],
                        ALU.is_equal, ALU.mult), reads=[rt_t[t], cst_t], writes=[Sel_t[t]])
                for dc in range(8):
                    ps, pt = C.psum()
                    C.mm(ps[:, 0:NBH], [(htm[:, t, dc * 128:(dc + 1) * 128], Sel[:, t, :]) for t in range(NT)], htm_t + Sel_t, [pt])
                    S.op("act", lambda e, ps=ps, dc=dc, nb=nb: e.copy(hgT[:, dc, nb * NBH:(nb + 1) * NBH], ps[:, 0:NBH]),
                         reads=[pt], writes=[hg_t[nb]])
            handles = pre_issued
            pre_issued = []
            for s in range(11):
                if handles:
                    (wg_b, wg_t), (wu_b, wu_t) = handles[0], handles[1]
                    handles = handles[2:]
                else:
                    wg_b, wg_t = ws.issue(wgu_slab(Wg, ex_i, s), [128, 8, 256], "pool")
                    wu_b, wu_t = ws.issue(wgu_slab(Wu, ex_i, s), [128, 8, 256], "pool")
                for j in range(2):
                    ff = 2 * s + j
                    for nb in range(2):
                        sl = slice(nb * NBH, (nb + 1) * NBH)
                        pg, pgt = C.psum()
                        C.mm(pg[:, 0:NBH], [(wg_b[:, c, j * 128:(j + 1) * 128], hgT[:, c, sl]) for c in range(8)], [wg_t, hg_t[nb]], [pgt])
                        pu, put = C.psum()
                        C.mm(pu[:, 0:NBH], [(wu_b[:, c, j * 128:(j + 1) * 128], hgT[:, c, sl]) for c in range(8)], [wu_t, hg_t[nb]], [put])
                        sgi = ksg % 2; ksg += 1
                        S.op("act", lambda e, sgi=sgi, pg=pg: e.activation(sg[sgi], pg[:, 0:NBH], AF.Silu), reads=[pgt], writes=[sg_t[sgi]])
                        S.op("dve", lambda e, sgi=sgi, pu=pu, ff=ff, sl=sl: e.tensor_tensor(aT[:, ff, sl], sg[sgi], pu[:, 0:NBH], ALU.mult),
                             reads=[put, sg_t[sgi]], writes=[aT_t[ff]])
            for half in range(2):
                for s4 in range(6):
                    nf = 4 if s4 < 5 else 2
                    wd_b, wd_t = ws.issue(wd_slab(Wd, ex_i, half, s4, nf), [128, nf, 512], "act")

                    def dn(e, s4=s4, nf=nf, wd_b=wd_b):
                        inst = None
                        for j in range(nf):
                            ff = 4 * s4 + j
                            for st_ in range(NSL):
                                inst = e.matmul(C.banks[st_], aT[:, ff, st_ * 128:(st_ + 1) * 128], wd_b[:, j, :],
                                                start=(ff == 0), stop=(ff == NFF - 1))
                        return inst
                    S.op("pe", dn, reads=[wd_t] + aT_t[4 * s4:4 * s4 + nf], writes=C.bank_t[0:NSL])
                for st_ in range(NSL):
                    S.op("act", lambda e, st_=st_, half=half: e.copy(ye[:, st_, half * 512:(half + 1) * 512], C.banks[st_]),
                         reads=[C.bank_t[st_]], writes=[ye_t[st_]])
            if ex_i + 1 < NE:
                pre_issued = [ws.issue(wgu_slab(Wg, ex_i + 1, 0), [128, 8, 256], "pool"),
                              ws.issue(wgu_slab(Wu, ex_i + 1, 0), [128, 8, 256], "pool")]
            for tp in range(8):
                i = ktp % 2; ktp += 1
                pp, ppt = C.psum()
                pgp, pgpt = C.psum()
                for q in range(2):
                    t = 2 * tp + q
                    d = kd % 2; kd += 1
                    S.op("dve", lambda e, d=d, t=t, ex_i=ex_i: e.tensor_scalar(dgp[d], C.ident_f, pos_sb[:, t, ex_i:ex_i + 1], None, ALU.mult),
                         reads=[rt_t[t], C.ct], writes=[dg_t[d]])
                    S.op("dve", lambda e, d=d, t=t, ex_i=ex_i: e.tensor_scalar(dgg[d], C.ident_f, G_sb[:, t, ex_i:ex_i + 1], None, ALU.mult),
                         reads=[rt_t[t], C.ct, dg_t[d]], writes=[dg_t[d]])
                    C.mm(pp[:, q * 128:(q + 1) * 128], [(C.ones_f, dgp[d])], [dg_t[d], C.ct], [ppt])
                    C.mm(pgp[:, q * 128:(q + 1) * 128], [(C.ones_f, dgg[d])], [dg_t[d], C.ct], [pgpt])
                S.op("act", lambda e, i=i, pgp=pgp: e.copy(grep[i], pgp[:, 0:256]), reads=[pgpt], writes=[grep_t[i]])
                for j in range(NSL):
                    S.op("dve", lambda e, i=i, j=j, pp=pp: e.scalar_tensor_tensor(
                        SelGT[i][:, j, :], pp[:, 0:256], slotcol[:, j:j + 1], grep[i], ALU.is_equal, ALU.mult),
                        reads=[ppt, grep_t[i], cst_t], writes=[SelGT_t[i]])
                S.dma("sp", ystg[i], xout_v[:, 2 * tp:2 * tp + 2, :], reads=[xo_t[2 * tp], xo_t[2 * tp + 1]], writes=[ystg_t[i]])
                for q in range(2):
                    for half in range(2):
                        ps, pt = C.psum()
                        C.mm(ps, [(SelGT[i][:, j, q * 128:(q + 1) * 128], ye[:, j, half * 512:(half + 1) * 512]) for j in range(NSL)],
                             [SelGT_t[i]] + ye_t, [pt])
                        S.op("dve", lambda e, i=i, q=q, half=half, ps=ps: e.tensor_tensor(
                            ystg[i][:, q, half * 512:(half + 1) * 512], ystg[i][:, q, half * 512:(half + 1) * 512], ps, ALU.add),
                            reads=[pt, ystg_t[i]], writes=[ystg_t[i]])
                S.dma("sp", xout_v[:, 2 * tp:2 * tp + 2, :], ystg[i], reads=[ystg_t[i]], writes=[xo_t[2 * tp], xo_t[2 * tp + 1]])


SCALE = 128.0 ** -0.5
GROUPS = ((128, 1), (512, 4), (2048, 16))
NEG = -80.0


class HeadNorm:
    def __init__(self, C):
        self.C = C
        self.raw = [C.alloc([128, 512]) for _ in range(2)]; self.raw_t = TL(2)
        self.sq = [C.alloc([128, 512]) for _ in range(2)]; self.sq_t = TL(2)
        self.rs = [C.alloc([128, 512]) for _ in range(2)]; self.rs_t = TL(2)
        self.k = 0

    def run(self, ps, pt, n, g_col, g_t, out_ap, out_writes):
        C = self.C; S = C.S
        i = self.k % 2; self.k += 1
        raw = self.raw[i][:, 0:n]; sq = self.sq[i][:, 0:n]; rs = self.rs[i][:, 0:n]
        S.op("dve", lambda e: e.tensor_copy(raw, ps[:, 0:n]), reads=[pt], writes=[self.raw_t[i]])
        S.op("act", lambda e: e.activation(sq, raw, AF.Square), reads=[self.raw_t[i]], writes=[self.sq_t[i]])
        p2, p2t = C.psum()
        C.mm(p2[:, 0:n], [(C.ones_f, sq)], [self.sq_t[i], C.ct], [p2t])
        S.op("act", lambda e: e.activation(sq, p2[:, 0:n], AF.Ln, bias=EPS, scale=1.0 / 128), reads=[p2t], writes=[self.sq_t[i]])
        S.op("act", lambda e: e.activation(rs, sq, AF.Exp, scale=-0.5), reads=[self.sq_t[i]], writes=[self.rs_t[i]])
        S.op("dve", lambda e: e.scalar_tensor_tensor(out_ap, raw, g_col, rs, ALU.mult, ALU.mult),
             reads=[self.raw_t[i], self.rs_t[i], g_t], writes=out_writes)


def mem_kv(C, mem_ap, gmem_ap, wkv_ap, gk_col, gk_t, kT, v, kv_t):
    S = C.S
    with C.scope():
        mT = C.alloc([128, 8, MEM], ADT); mT_t = TL(2)
        norm_T(C, mem_ap, MEM, gmem_ap, mT, mT_t)
        ws = WStream(C, nstage=2, nslot=2)
        hn = HeadNorm(C)
        for sl in range(2):
            wb, wt = ws.issue(wview(wkv_ap, 0, 8, sl * 256, 256), [128, 8, 256], "pool")
            for j in range(2):
                h = sl * 2 + j
                ps, pt = C.psum()
                C.mm(ps[:, 0:MEM], [(wb[:, c, j * 128:(j + 1) * 128], mT[:, c, :]) for c in range(8)], [wt] + mT_t, [pt])
                hn.run(ps, pt, MEM, gk_col, gk_t, kT[:, h, :], [kv_t])
        for sl in range(2):
            wb, wt = ws.issue(wview(wkv_ap, 0, 8, 512 + sl * 256, 256), [128, 8, 256], "pool")
            for mc in range(2):
                ps, pt = C.psum()
                C.mm(ps[:, 0:256], [(mT[:, c, mc * 128:(mc + 1) * 128], wb[:, c, :]) for c in range(8)], [wt] + mT_t, [pt])
                S.op("act", lambda e, ps=ps, mc=mc, sl=sl: e.copy(v[:, mc, sl * 256:(sl + 1) * 256], ps[:, 0:256]),
                     reads=[pt], writes=[kv_t])


class CrossAttn:
    def __init__(self, C):
        self.C = C
        self.pT = [C.alloc([128, 2, 512], ADT) for _ in range(2)]; self.pT_t = TL(2)
        self.rd = [C.alloc([128, 512]) for _ in range(2)]; self.rd_t = TL(2)
        self.k = 0

    def run(self, h, tg, q_ap, q_reads, kT, v, kv_t, out_ap, out_writes):
        C = self.C; S = C.S
        i = self.k % 2; self.k += 1
        pT = self.pT[i]; rd = self.rd[i]
        for mc in range(2):
            ps, pt = C.psum()
            C.mm(ps, [(kT[:, h, mc * 128:(mc + 1) * 128], q_ap)], [kv_t] + q_reads, [pt])
            S.op("act", lambda e, ps=ps, mc=mc: e.activation(pT[:, mc, :], ps, AF.Exp, scale=SCALE), reads=[pt], writes=[self.pT_t[i]])
        pd, pdt = C.psum()
        C.mm(pd, [(C.ones_b, pT[:, 0, :]), (C.ones_b, pT[:, 1, :])], [self.pT_t[i], C.ct], [pdt])
        po, pot = C.psum()
        C.mm(po, [(v[:, 0, h * 128:(h + 1) * 128], pT[:, 0, :]), (v[:, 1, h * 128:(h + 1) * 128], pT[:, 1, :])],
             [self.pT_t[i], kv_t], [pot])
        S.op("act", lambda e: e.activation(rd, pd, AF.Ln), reads=[pdt], writes=[self.rd_t[i]])
        S.op("act", lambda e: e.activation(rd, rd, AF.Exp, scale=-1.0), reads=[self.rd_t[i]], writes=[self.rd_t[i]])
        S.op("dve", lambda e: e.tensor_tensor(out_ap, po, rd, ALU.mult), reads=[pot, self.rd_t[i]], writes=out_writes)


def out_proj(C, mixT, mixT_t, wout_ap, x_in, x_out):
    S = C.S
    with C.scope():
        ws = WStream(C, nstage=3, nslot=4)
        yacc = [C.alloc([128, 4, 1024], F32) for _ in range(2)]; yacc_t = [TL(8), TL(8)]
        xin_v = x_in.rearrange("(t p) d -> p t d", p=128)
        xout_v = x_out.rearrange("(t p) d -> p t d", p=128)
        for tg in range(4):
            ya = yacc[tg % 2]; ya_t = yacc_t[tg % 2]
            S.dma("sp", ya, xin_v[:, tg * 4:(tg + 1) * 4, :], writes=ya_t)
            for s in range(4):
                wb, wt = ws.issue(wview(wout_ap, 2 * s, 2, 0, 1024), [128, 2, 1024], "act")

                def dn(e, s=s, wb=wb, tg=tg):
                    inst = None
                    for j in range(2):
                        k = 2 * s + j
                        for tt in range(4):
                            t0 = tg * 512 + tt * 128
                            for half in range(2):
                                inst = e.matmul(C.banks[tt * 2 + half], mixT[:, k, t0:t0 + 128],
                                                wb[:, j, half * 512:(half + 1) * 512], start=(k == 0), stop=(k == 7))
                    return inst
                S.op("pe", dn, reads=[wt, mixT_t[2 * s][tg], mixT_t[2 * s + 1][tg]], writes=C.bank_t)
            for tt in range(4):
                for half in range(2):
                    b = tt * 2 + half
                    S.op("dve", lambda e, b=b, tt=tt, half=half, ya=ya: e.tensor_tensor(
                        ya[:, tt, half * 512:(half + 1) * 512], ya[:, tt, half * 512:(half + 1) * 512], C.banks[b], ALU.add),
                        reads=[C.bank_t[b], ya_t[b]], writes=[ya_t[b]])
            S.dma("sp", xout_v[:, tg * 4:(tg + 1) * 4, :], ya, reads=ya_t)


def load_col(C, ap128):
    t = T()
    sb = C.alloc([128, 1])
    C.S.dma("sp", sb, ap128, writes=[t])
    return sb, t


def stage_mix1(C, x_in, x_out, mem, W):
    S = C.S
    Win = W["at_w_in"]
    with C.scope():
        gk_col, gk_t = load_col(C, W["xk_norm1"])
        gxq_col, gxq_t = load_col(C, W["xq_norm1"])
        gaq_col, gaq_t = load_col(C, W["at_q_norm"])
        gak_col, gak_t = load_col(C, W["at_k_norm"])
        kT = C.alloc([128, 4, MEM], ADT); v = C.alloc([128, 2, 512], ADT); kv_t = T()
        mem_kv(C, mem, W["norm_mem1"], W["w_mem_kv1"], gk_col, gk_t, kT, v, kv_t)
        mixT = C.alloc([128, 8, TOK], ADT); mixT_t = [TL(4) for _ in range(8)]
        with C.scope():
            hT = C.alloc([128, 8, TOK], ADT); hT_t = TL(NT)
            norm_T(C, x_in, TOK, W["norm_mix1"], hT, hT_t)
            ws = WStream(C, nstage=3, nslot=4, elems=1024)
            hn = HeadNorm(C)
            ca = CrossAttn(C)
            qh = C.alloc([128, TOK], ADT); qh_t = TL(4)
            for h in range(4):
                wb, wt = ws.issue(wview(Win, 0, 8, 4608 + h * 128, 128), [128, 8, 128], "pool")
                for tg in range(4):
                    tok = slice(tg * 512, (tg + 1) * 512)
                    ps, pt = C.psum()
                    C.mm(ps, [(wb[:, c, :], hT[:, c, tok]) for c in range(8)], [wt] + hT_t[tg * 4:tg * 4 + 4], [pt])
                    hn.run(ps, pt, 512, gxq_col, gxq_t, qh[:, tok], [qh_t[tg]])
                    ca.run(h, tg, qh[:, tok], [qh_t[tg]], kT, v, kv_t, mixT[:, 4 + h, tok], [mixT_t[4 + h][tg]])
            BT = C.alloc([128, 12, 256], F32); BT_t = TL(12)
            tab = C.alloc([128, 32 * 12], F32); tab_t = T()
            S.dma("sp", tab, W["rel_bias"].rearrange("b h -> (b h)").partition_broadcast(128), writes=[tab_t])
            negm = C.alloc([128, 256], F32); negm_t = T()
            S.dma("sp", negm, W["c_negmask"], writes=[negm_t])
            for gh in range(12):
                S.op("pool", lambda e, gh=gh: e.tensor_copy(BT[:, gh, :], negm), reads=[negm_t], writes=[BT_t[gh]])
            oh = [C.alloc([128, 256], F32) for _ in range(2)]; oh_t = TL(2)
            kk = 0
            for g in range(3):
                for b in W["_buckets"][g]:
                    o = oh[kk % 2]; ot = oh_t[kk % 2]; kk += 1
                    S.dma("sp", o, W["c_oh"][g, b], writes=[ot])
                    for h in range(4):
                        gh = g * 4 + h
                        S.op("dve", lambda e, o=o, gh=gh, b=b: e.scalar_tensor_tensor(
                            BT[:, gh, :], o, tab[:, b * 12 + gh:b * 12 + gh + 1], BT[:, gh, :], ALU.mult, ALU.add),
                            reads=[ot, tab_t, BT_t[gh]], writes=[BT_t[gh]])
            acc_o = C.alloc([128, TOK], F32); acc_d = C.alloc([128, TOK], F32); acc_t = T()
            qg = C.alloc([128, TOK], ADT); qg_t = TL(4)
            kg = C.alloc([128, TOK], ADT); kg_t = TL(4)
            vg = C.alloc([128, 16, 128], ADT); vg_t = TL(16)
            tS = [C.alloc([128, 256], F32) for _ in range(2)]; tS_t = TL(2)
            pT = [C.alloc([128, 256], ADT) for _ in range(2)]; pT_t = TL(2)
            kc = 0
            for h in range(4):
                S.op("pool", lambda e: e.memset(acc_o, 0.0), writes=[acc_t])
                S.op("pool", lambda e: e.memset(acc_d, 0.0), writes=[acc_t])
                for g, (win, r) in enumerate(GROUPS):
                    L = TOK // r
                    nj = L // 128
                    cq = ((0 * 3 + g) * 4 + h) * 128
                    ck = ((1 * 3 + g) * 4 + h) * 128
                    cv = ((2 * 3 + g) * 4 + h) * 128
                    wq, wqt = ws.issue(wview(Win, 0, 8, cq, 128), [128, 8, 128], "pool")
                    for tg in range(4):
                        tok = slice(tg * 512, (tg + 1) * 512)
                        ps, pt = C.psum()
                        C.mm(ps, [(wq[:, c, :], hT[:, c, tok]) for c in range(8)], [wqt] + hT_t[tg * 4:tg * 4 + 4], [pt])
                        hn.run(ps, pt, 512, gaq_col, gaq_t, qg[:, tok], [qg_t[tg]])
                    wk, wkt = ws.issue(wview(Win, 0, 8, ck, 128), [128, 8, 128], "pool")
                    for tg in range(4):
                        tok = slice(tg * 512, (tg + 1) * 512)
                        ps, pt = C.psum()
                        C.mm(ps, [(wk[:, c, :], hT[:, c, tok]) for c in range(8)], [wkt] + hT_t[tg * 4:tg * 4 + 4], [pt])
                        hn.run(ps, pt, 512, gak_col, gak_t, kg[:, tok], [kg_t[tg]])
                    wv, wvt = ws.issue(wview(Win, 0, 8, cv, 128), [128, 8, 128], "pool")
                    for r0 in range(r):
                        for j in range(nj):
                            idx = r0 * nj + j
                            a0 = r0 + r * 128 * j
                            a1 = a0 + r * 127 + 1
                            ps, pt = C.psum()
                            C.mm(ps[:, 0:128], [(hT[:, c, a0:a1:r], wv[:, c, :]) for c in range(8)], [wvt] + hT_t, [pt])
                            S.op("act", lambda e, ps=ps, idx=idx: e.copy(vg[:, idx, :], ps[:, 0:128]), reads=[pt], writes=[vg_t[idx]])
                    for r0 in range(r):
                        for j in range(nj):
                            idx = r0 * nj + j
                            a0 = r0 + r * 128 * j
                            a1 = a0 + r * 127 + 1
                            q0 = max(0, 128 * j - 64); q1 = min(L, 128 * j + 192)
                            N = q1 - q0
                            n0 = q0 - (128 * j - 64)
                            b0 = r0 + r * q0
                            b1 = b0 + r * (N - 1) + 1
                            i = kc % 2; kc += 1
                            ps, pt = C.psum()
                            C.mm(ps[:, 0:N], [(kg[:, a0:a1:r], qg[:, b0:b1:r])], kg_t + qg_t, [pt])
                            S.op("dve", lambda e, ps=ps, i=i, N=N, n0=n0, g=g, h=h: e.scalar_tensor_tensor(
                                tS[i][:, 0:N], ps[:, 0:N], SCALE, BT[:, g * 4 + h, n0:n0 + N], ALU.mult, ALU.add),
                                reads=[pt, BT_t[g * 4 + h]], writes=[tS_t[i]])
                            S.op("act", lambda e, i=i, N=N: e.activation(pT[i][:, 0:N], tS[i][:, 0:N], AF.Exp),
                                 reads=[tS_t[i]], writes=[pT_t[i]])
                            pd, pdt = C.psum()
                            C.mm(pd[:, 0:N], [(C.ones_b, pT[i][:, 0:N])], [pT_t[i], C.ct], [pdt])
                            po, pot = C.psum()
                            C.mm(po[:, 0:N], [(vg[:, idx, :], pT[i][:, 0:N])], [pT_t[i], vg_t[idx]], [pot])
                            S.op("dve", lambda e, pd=pd, N=N, b0=b0, b1=b1, r=r: e.tensor_tensor(
                                acc_d[:, b0:b1:r], acc_d[:, b0:b1:r], pd[:, 0:N], ALU.add), reads=[pdt, acc_t], writes=[acc_t])
                            S.op("dve", lambda e, po=po, N=N, b0=b0, b1=b1, r=r: e.tensor_tensor(
                                acc_o[:, b0:b1:r], acc_o[:, b0:b1:r], po[:, 0:N], ALU.add), reads=[pot, acc_t], writes=[acc_t])
                S.op("act", lambda e: e.activation(acc_d, acc_d, AF.Ln), reads=[acc_t], writes=[acc_t])
                S.op("act", lambda e: e.activation(acc_d, acc_d, AF.Exp, scale=-1.0), reads=[acc_t], writes=[acc_t])
                for tg in range(4):
                    tok = slice(tg * 512, (tg + 1) * 512)
                    S.op("dve", lambda e, tok=tok, h=h: e.tensor_tensor(mixT[:, h, tok], acc_o[:, tok], acc_d[:, tok], ALU.mult),
                         reads=[acc_t], writes=[mixT_t[h][tg]])
        out_proj(C, mixT, mixT_t, W["w_out1"], x_in, x_out)


NFFT = 4096
WSC = 2.0 / NFFT


def hyena_filter(C, W, o, h2T, h2_t, pnyq, pnyq_t, altcol, alt_t):
    S = C.S
    with C.scope():
        w3o = C.alloc([64, 1024], F32); w3_t = T()
        S.dma("sp", w3o, W["hy_filt_w3"][:, o * 1024:(o + 1) * 1024], writes=[w3_t])
        Gp = C.alloc([128, 16, 512], ADT); Gm = C.alloc([128, 16, 512], ADT); G_t = TL(16)
        dk = [C.alloc([128, 512], F32) for _ in range(2)]; dk_t = TL(2)
        kf = [C.alloc([128, 512], F32) for _ in range(2)]; kb = [C.alloc([128, 512], F32) for _ in range(2)]; kk_t = TL(2)
        ab = [C.alloc([128, 2, 512], ADT) for _ in range(2)]; ab_t = TL(2)
        nb = C.reserve()
        nps, npt = C.banks[nb], C.bank_t[nb]
        for dc in range(16):
            i = dc % 2
            S.dma("sp", dk[i], W["c_decay"][dc * 128:(dc + 1) * 128, :], writes=[dk_t[i]])
            pf, pft = C.psum()
            C.mm(pf, [(h2T[:, dc * 128:(dc + 1) * 128], w3o[:, 0:512])], [h2_t, w3_t], [pft])
            pb, pbt = C.psum()
            C.mm(pb, [(h2T[:, dc * 128:(dc + 1) * 128], w3o[:, 512:1024])], [h2_t, w3_t], [pbt])
            S.op("dve", lambda e, i=i, pf=pf: e.tensor_tensor(kf[i], pf, dk[i], ALU.mult), reads=[pft, dk_t[i]], writes=[kk_t[i]])
            S.op("dve", lambda e, i=i, pb=pb: e.tensor_tensor(kb[i], pb, dk[i], ALU.mult), reads=[pbt, dk_t[i], kk_t[i]], writes=[kk_t[i]])
            if dc == 0:
                S.op("dve", lambda e, i=i: e.memset(kb[i][0:1, :], 0.0), reads=[kk_t[i]], writes=[kk_t[i]])
            S.op("pool", lambda e, i=i, dc=dc: e.tensor_tensor(Gp[:, dc, :], kf[i], kb[i], ALU.add), reads=[kk_t[i]], writes=[G_t[dc]])
            S.op("pool", lambda e, i=i, dc=dc: e.tensor_tensor(Gm[:, dc, :], kf[i], kb[i], ALU.subtract), reads=[kk_t[i], G_t[dc]], writes=[G_t[dc]])
            S.op("act", lambda e, i=i: e.activation(ab[i][:, 0, :], kf[i], AF.Abs), reads=[kk_t[i]], writes=[ab_t[i]])
            S.op("act", lambda e, i=i: e.activation(ab[i][:, 1, :], kb[i], AF.Abs), reads=[kk_t[i], ab_t[i]], writes=[ab_t[i]])

            def nm(e, i=i, dc=dc):
                e.matmul(nps, C.ones_b, ab[i][:, 0, :], start=(dc == 0), stop=False)
                return e.matmul(nps, C.ones_b, ab[i][:, 1, :], start=False, stop=(dc == 15))
            S.op("pe", nm, reads=[ab_t[i], C.ct], writes=[npt])
        rn = C.alloc([128, 512], F32); sk = C.alloc([128, 512], F32); rn_t = T()
        S.dma("sp", sk, W["hy_skip"][o].partition_broadcast(128), writes=[rn_t])
        S.op("dve", lambda e: e.tensor_scalar(rn, nps, 1e-6, None, ALU.add), reads=[npt], writes=[rn_t])
        S.op("dve", lambda e: e.reciprocal(rn, rn), reads=[rn_t], writes=[rn_t])
        S.op("dve", lambda e: e.tensor_scalar(rn, rn, WSC, None, ALU.mult), reads=[rn_t], writes=[rn_t])
        S.op("dve", lambda e: e.tensor_scalar(sk, sk, WSC, None, ALU.mult), reads=[rn_t], writes=[rn_t])
        C.release(nb)
        if HY_F <= 1:
            return
        pn, pnt = C.psum()
        C.mm(pn, [(altcol, Gp[:, dc, :]) for dc in range(16)], G_t + [alt_t], [pnt])
        S.op("dve", lambda e: e.tensor_tensor(pnyq[0:1, o, :], pn[0:1, :], rn[0:1, :], ALU.mult), reads=[pnt, rn_t], writes=[pnyq_t])
        S.op("dve", lambda e: e.tensor_tensor(pnyq[0:1, o, :], pnyq[0:1, o, :], sk[0:1, :], ALU.add), reads=[pnyq_t, rn_t], writes=[pnyq_t])
        S.op("dve", lambda e: e.tensor_scalar(pnyq[0:1, o, :], pnyq[0:1, o, :], 0.5, None, ALU.mult), reads=[pnyq_t], writes=[pnyq_t])
        if HY_F <= 2:
            return
        tb = [C.alloc([128, 16, 256], ADT) for _ in range(4)]; tb_t = TL(4)
        tmp = [C.alloc([128, 512], F32) for _ in range(2)]; tmp_t = TL(2)
        st = [C.alloc([128, 512], ADT) for _ in range(4)]; st_t = TL(4)
        k = 0
        for fb in range(8):
            cb, cbt = tb[(2 * fb) % 4], tb_t[(2 * fb) % 4]
            sb, sbt = tb[(2 * fb + 1) % 4], tb_t[(2 * fb + 1) % 4]
            S.dma("sp", cb, W["c_dftc"][fb], writes=[cbt])
            S.dma("sp", sb, W["c_dfts"][fb], writes=[sbt])
            for j in range(2):
                fc = 2 * fb + j
                pp, ppt = C.psum()
                C.mm(pp, [(cb[:, dc, j * 128:(j + 1) * 128], Gp[:, dc, :]) for dc in range(16)], G_t + [cbt], [ppt])
                pq, pqt = C.psum()
                C.mm(pq, [(sb[:, dc, j * 128:(j + 1) * 128], Gm[:, dc, :]) for dc in range(16)], G_t + [sbt], [pqt])
                i = k % 2; a = (2 * k) % 4; b = (2 * k + 1) % 4; k += 1
                S.op("dve", lambda e, i=i, pp=pp: e.tensor_tensor(tmp[i], pp, rn, ALU.mult), reads=[ppt, rn_t], writes=[tmp_t[i]])
                S.op("dve", lambda e, i=i, a=a: e.tensor_tensor(st[a], tmp[i], sk, ALU.add), reads=[tmp_t[i], rn_t], writes=[st_t[a]])
                if fc == 0:
                    S.op("dve", lambda e, a=a: e.tensor_scalar(st[a][0:1, :], st[a][0:1, :], 0.5, None, ALU.mult), reads=[st_t[a]], writes=[st_t[a]])
                S.op("dve", lambda e, b=b, pq=pq: e.tensor_tensor(st[b], pq, rn, ALU.mult), reads=[pqt, rn_t], writes=[st_t[b]])
                S.dma("sp", W["hy_pq"][o, 0, fc], st[a], reads=[st_t[a]], writes=[W["hy_pq_t"]])
                S.dma("sp", W["hy_pq"][o, 1, fc], st[b], reads=[st_t[b]], writes=[W["hy_pq_t"]])


def hyena_conv(C, W, o, u, u_t, pnyq, pnyq_t, altcol, altrow, alt_t, out_tok=None, out_tok_t=None, mixT=None, mixT_t=None):
    S = C.S
    with C.scope():
        tb = [C.alloc([128, 16, 256], ADT) for _ in range(4)]; tb_t = TL(4)
        pqs = [C.alloc([128, 2, 2, 512], ADT) for _ in range(2)]; pqs_t = TL(2)
        Rr = C.alloc([128, 16, 512], ADT); Ri = C.alloc([128, 16, 512], ADT); R_t = TL(16)
        t1 = [C.alloc([128, 512], F32) for _ in range(2)]; t2 = [C.alloc([128, 512], F32) for _ in range(2)]; tt_t = TL(2)
        rny = C.alloc([128, 512], ADT); rny_t = T()
        S.op("pool", lambda e: e.memset(rny, 0.0), writes=[rny_t])
        gt = [C.alloc([128, 512], ADT) for _ in range(2)]; gt_t = TL(2)
        k = 0
        for fb in range(8):
            cb, cbt = tb[(2 * fb) % 4], tb_t[(2 * fb) % 4]
            sb, sbt = tb[(2 * fb + 1) % 4], tb_t[(2 * fb + 1) % 4]
            S.dma("sp", cb, W["c_dftc"][fb], writes=[cbt])
            S.dma("sp", sb, W["c_dfts"][fb], writes=[sbt])
            pq = pqs[fb % 2]; pq_t = pqs_t[fb % 2]
            S.dma("sp", pq[:, 0], W["hy_pq"][o, 0, 2 * fb:2 * fb + 2].rearrange("f p c -> p f c"), reads=[W["hy_pq_t"]], writes=[pq_t])
            S.dma("sp", pq[:, 1], W["hy_pq"][o, 1, 2 * fb:2 * fb + 2].rearrange("f p c -> p f c"), reads=[W["hy_pq_t"]], writes=[pq_t])
            for j in range(2):
                fc = 2 * fb + j
                pa, pat = C.psum()
                C.mm(pa, [(cb[:, sc, j * 128:(j + 1) * 128], u[:, sc, :]) for sc in range(16)], u_t + [cbt], [pat])
                pb, pbt = C.psum()
                C.mm(pb, [(sb[:, sc, j * 128:(j + 1) * 128], u[:, sc, :]) for sc in range(16)], u_t + [sbt], [pbt])
                i = k % 2; k += 1
                P = pq[:, 0, j, :]; Q = pq[:, 1, j, :]
                S.op("dve", lambda e, i=i, pa=pa, P=P: e.tensor_tensor(t1[i], pa, P, ALU.mult), reads=[pat, pq_t], writes=[tt_t[i]])
                S.op("dve", lambda e, i=i, pb=pb, Q=Q: e.tensor_tensor(t2[i], pb, Q, ALU.mult), reads=[pbt, pq_t, tt_t[i]], writes=[tt_t[i]])
                S.op("pool", lambda e, i=i, fc=fc: e.tensor_tensor(Rr[:, fc, :], t1[i], t2[i], ALU.subtract), reads=[tt_t[i]], writes=[R_t[fc]])
                S.op("dve", lambda e, i=i, pa=pa, Q=Q: e.tensor_tensor(t1[i], pa, Q, ALU.mult), reads=[pat, pq_t, tt_t[i]], writes=[tt_t[i]])
                S.op("dve", lambda e, i=i, pb=pb, P=P: e.tensor_tensor(t2[i], pb, P, ALU.mult), reads=[pbt, pq_t, tt_t[i]], writes=[tt_t[i]])
                S.op("pool", lambda e, i=i, fc=fc: e.tensor_tensor(Ri[:, fc, :], t1[i], t2[i], ALU.add), reads=[tt_t[i], R_t[fc]], writes=[R_t[fc]])
        pn, pnt = C.psum()
        C.mm(pn, [(altcol, u[:, sc, :]) for sc in range(16)], u_t + [alt_t], [pnt])
        S.op("dve", lambda e: e.tensor_tensor(rny[0:1, :], pn[0:1, :], pnyq[0:1, o, :], ALU.mult), reads=[pnt, pnyq_t, rny_t], writes=[rny_t])
        for tbk in range(8):
            cb, cbt = tb[(2 * tbk) % 4], tb_t[(2 * tbk) % 4]
            sb, sbt = tb[(2 * tbk + 1) % 4], tb_t[(2 * tbk + 1) % 4]
            S.dma("sp", cb, W["c_dftc"][tbk], writes=[cbt])
            S.dma("sp", sb, W["c_dfts"][tbk], writes=[sbt])
            if o == 0:
                for j in range(2):
                    tc = 2 * tbk + j
                    g = gt[tc % 2]; g_t = gt_t[tc % 2]
                    S.dma("sp", g, W["hy_c1"][tc], reads=[W["hy_c1_t"]], writes=[g_t])
                    ps, pt = C.psum()
                    pairs = [(cb[:, fc, j * 128:(j + 1) * 128], Rr[:, fc, :]) for fc in range(16)]
                    pairs += [(sb[:, fc, j * 128:(j + 1) * 128], Ri[:, fc, :]) for fc in range(16)]
                    pairs += [(altrow[:, 0:128], rny[:, :])]
                    C.mm(ps, pairs, R_t + [cbt, sbt, rny_t, alt_t], [pt])
                    S.op("dve", lambda e, ps=ps, g=g, tc=tc: e.tensor_tensor(out_tok[:, tc, :], ps, g, ALU.mult),
                         reads=[pt, g_t], writes=[out_tok_t[tc]])
            else:
                for cc in range(4):
                    g = gt[cc % 2]; g_t = gt_t[cc % 2]
                    S.dma("sp", g[:, 0:256], W["hy_c2T"][cc][:, tbk * 256:(tbk + 1) * 256], reads=[W["hy_c2T_t"]], writes=[g_t])
                    ps, pt = C.psum()
                    pairs = [(Rr[:, fc, cc * 128:(cc + 1) * 128], cb[:, fc, :]) for fc in range(16)]
                    pairs += [(Ri[:, fc, cc * 128:(cc + 1) * 128], sb[:, fc, :]) for fc in range(16)]
                    pairs += [(rny[:, cc * 128:(cc + 1) * 128], altrow[:, 0:256])]
                    C.mm(ps[:, 0:256], pairs, R_t + [cbt, sbt, rny_t, alt_t], [pt])
                    S.op("dve", lambda e, ps=ps, g=g, cc=cc, tbk=tbk: e.tensor_tensor(
                        mixT[:, cc, tbk * 256:(tbk + 1) * 256], ps[:, 0:256], g[:, 0:256], ALU.mult),
                        reads=[pt, g_t], writes=[mixT_t[cc][tbk // 2]])


def stage_mix0(C, x_in, x_out, mem, W):
    S = C.S
    Win = W["hy_w_in"]
    for nm in ("hy_pq", "hy_c1", "hy_c2T", "hy_u0"):
        W[nm + "_t"] = T()
    with C.scope():
        gk_col, gk_t = load_col(C, W["xk_norm0"])
        gxq_col, gxq_t = load_col(C, W["xq_norm0"])
        kT = C.alloc([128, 4, MEM], ADT); v = C.alloc([128, 2, 512], ADT); kv_t = T()
        mem_kv(C, mem, W["norm_mem0"], W["w_mem_kv0"], gk_col, gk_t, kT, v, kv_t)
        mixT = C.alloc([128, 8, TOK], ADT); mixT_t = [TL(4) for _ in range(8)]
        altcol = C.alloc([128, 128], ADT); altrow = C.alloc([128, 512], ADT); alt_t = T()
        altf = C.alloc([128, 128], F32); altrf = C.alloc([128, 512], F32)
        S.dma("sp", altf, W["c_altcol"], writes=[alt_t])
        S.dma("sp", altrf, W["c_altrow"], writes=[alt_t])
        S.op("dve", lambda e: e.tensor_copy(altcol, altf), reads=[alt_t], writes=[alt_t])
        S.op("dve", lambda e: e.tensor_copy(altrow, altrf), reads=[alt_t], writes=[alt_t])
        pnyq = C.alloc([1, 2, 512], F32); pnyq_t = T()
        with C.scope():
            hT = C.alloc([128, 8, TOK], ADT); hT_t = TL(NT)
            norm_T(C, x_in, TOK, W["norm_mix0"], hT, hT_t)
            ws = WStream(C, nstage=3, nslot=4, elems=1024)
            hn = HeadNorm(C)
            ca = CrossAttn(C)
            qh = C.alloc([128, TOK], ADT); qh_t = TL(4)
            for h in range(4):
                wb, wt = ws.issue(wview(Win, 0, 8, 1536 + h * 128, 128), [128, 8, 128], "pool")
                for tg in range(4):
                    tok = slice(tg * 512, (tg + 1) * 512)
                    ps, pt = C.psum()
                    C.mm(ps, [(wb[:, c, :], hT[:, c, tok]) for c in range(8)], [wt] + hT_t[tg * 4:tg * 4 + 4], [pt])
                    hn.run(ps, pt, 512, gxq_col, gxq_t, qh[:, tok], [qh_t[tg]])
                    ca.run(h, tg, qh[:, tok], [qh_t[tg]], kT, v, kv_t, mixT[:, 4 + h, tok], [mixT_t[4 + h][tg]])
            cwb = C.alloc([128, 12, 4], F32); cwb_t = T()
            S.dma("sp", cwb, W["hy_cwb"], writes=[cwb_t])
            zp = [C.alloc([128, TOK + 2], F32) for _ in range(2)]; zp_t = TL(2)
            for i in range(2):
                S.op("pool", lambda e, i=i: e.memset(zp[i], 0.0), writes=[zp_t[i]])
            ctmp = C.alloc([128, TOK], F32); ctmp_t = T()
            cv = [C.alloc([128, TOK], ADT) for _ in range(2)]; cv_t = TL(2)
            stg = [C.alloc([128, 4, 128], ADT) for _ in range(2)]; stg_t = TL(2)
            kst = 0
            for ch in range(12):
                i = ch % 2
                wb, wt = ws.issue(wview(Win, 0, 8, ch * 128, 128), [128, 8, 128], "pool")
                for tg in range(4):
                    tok = slice(tg * 512, (tg + 1) * 512)
                    ps, pt = C.psum()
                    C.mm(ps, [(wb[:, c, :], hT[:, c, tok]) for c in range(8)], [wt] + hT_t[tg * 4:tg * 4 + 4], [pt])
                    S.op("act", lambda e, ps=ps, i=i, tg=tg: e.copy(zp[i][:, 1 + tg * 512:1 + (tg + 1) * 512], ps), reads=[pt], writes=[zp_t[i]])
                z = zp[i]
                S.op("dve", lambda e, z=z, ch=ch: e.tensor_scalar(ctmp, z[:, 1:TOK + 1], cwb[:, ch, 1:2], cwb[:, ch, 3:4], ALU.mult, ALU.add),
                     reads=[zp_t[i], cwb_t], writes=[ctmp_t])
                S.op("dve", lambda e, z=z, ch=ch: e.scalar_tensor_tensor(ctmp, z[:, 0:TOK], cwb[:, ch, 0:1], ctmp, ALU.mult, ALU.add),
                     reads=[zp_t[i], cwb_t, ctmp_t], writes=[ctmp_t])
                S.op("dve", lambda e, z=z, ch=ch, i=i: e.scalar_tensor_tensor(cv[i], z[:, 2:TOK + 2], cwb[:, ch, 2:3], ctmp, ALU.mult, ALU.add),
                     reads=[zp_t[i], cwb_t, ctmp_t], writes=[cv_t[i]])
                cc = ch % 4
                if ch >= 8:
                    S.dma("sp", W["hy_c2T"][cc], cv[i], reads=[cv_t[i]], writes=[W["hy_c2T_t"]])
                else:
                    dst = W["hy_u0"] if ch < 4 else W["hy_c1"]
                    dst_t = W["hy_u0_t"] if ch < 4 else W["hy_c1_t"]
                    for q4 in range(4):
                        ps, pt = C.psum()
                        psb = ps.bitcast(ADT)

                        def tr(e, psb=psb, i=i, q4=q4):
                            inst = None
                            for jj in range(4):
                                sc = q4 * 4 + jj
                                inst = e.transpose(psb[:, jj * 128:(jj + 1) * 128], cv[i][:, sc * 128:(sc + 1) * 128], C.ident_b)
                            return inst
                        S.op("pe", tr, reads=[cv_t[i], C.ct], writes=[pt])
                        si = kst % 2; kst += 1
                        S.op("act", lambda e, psb=psb, si=si: e.copy(stg[si], psb[:, 0:512].rearrange("p (a b) -> p a b", a=4)),
                             reads=[pt], writes=[stg_t[si]])
                        S.dma("sp", dst[q4 * 4:q4 * 4 + 4, :, cc * 128:(cc + 1) * 128].rearrange("s p c -> p s c"), stg[si],
                              reads=[stg_t[si]], writes=[dst_t])
        h2T = C.alloc([64, TOK], F32); h2_t = T()
        if HY_STOP <= 1:
            out_proj(C, mixT, mixT_t, W["w_out0"], x_in, x_out)
            return
        with C.scope():
            featsT = C.alloc([33, TOK], F32); f_t = T()
            S.dma("sp", featsT, W["c_featsT"], writes=[f_t])
            w1 = C.alloc([33, 64], F32); w2 = C.alloc([64, 64], F32); sm = C.alloc([64, 8], F32); sm_t = T()
            S.dma("sp", w1, W["hy_filt_w1"], writes=[sm_t])
            S.dma("sp", w2, W["hy_filt_w2"], writes=[sm_t])
            S.dma("sp", sm[:, 0:3], W["hy_fsm"], writes=[sm_t])
            S.op("dve", lambda e: e.tensor_tensor(sm[:, 3:4], sm[:, 0:1], sm[:, 1:2], ALU.mult), reads=[sm_t], writes=[sm_t])
            S.op("dve", lambda e: e.tensor_tensor(sm[:, 4:5], sm[:, 0:1], sm[:, 2:3], ALU.mult), reads=[sm_t], writes=[sm_t])
            h1T = C.alloc([64, TOK], F32); h1_t = T()
            pre = C.alloc([64, 512], F32); m1 = C.alloc([64, 512], F32); pre_t = T()
            PI = math.pi
            for layer in range(2):
                src, src_t, wl, dst, dst_t, fbcol = ((featsT, f_t, w1, h1T, h1_t, 3) if layer == 0 else (h1T, h1_t, w2, h2T, h2_t, 4))
                for tg in range(4):
                    tok = slice(tg * 512, (tg + 1) * 512)
                    ps, pt = C.psum()
                    C.mm(ps[0:64, :], [(wl, src[:, tok])], [sm_t, src_t], [pt])
                    S.op("dve", lambda e, ps=ps, fbcol=fbcol: e.tensor_scalar(pre, ps[0:64, :], sm[:, 0:1], sm[:, fbcol:fbcol + 1], ALU.mult, ALU.add),
                         reads=[pt, sm_t], writes=[pre_t])
                    for rep in range(2):
                        S.op("dve", lambda e: e.tensor_scalar(m1, pre, PI, -2 * PI, ALU.is_gt, ALU.mult), reads=[pre_t], writes=[pre_t])
                        S.op("dve", lambda e: e.tensor_tensor(pre, pre, m1, ALU.add), reads=[pre_t], writes=[pre_t])
                        S.op("dve", lambda e: e.tensor_scalar(m1, pre, -PI, 2 * PI, ALU.is_lt, ALU.mult), reads=[pre_t], writes=[pre_t])
                        S.op("dve", lambda e: e.tensor_tensor(pre, pre, m1, ALU.add), reads=[pre_t], writes=[pre_t])
                    S.op("act", lambda e, dst=dst, tok=tok: e.activation(dst[:, tok], pre, AF.Sin), reads=[pre_t], writes=[dst_t])
        if HY_STOP <= 2:
            out_proj(C, mixT, mixT_t, W["w_out0"], x_in, x_out)
            return
        hyena_filter(C, W, 0, h2T, h2_t, pnyq, pnyq_t, altcol, alt_t)
        if HY_STOP <= 3:
            out_proj(C, mixT, mixT_t, W["w_out0"], x_in, x_out)
            return
        u1 = C.alloc([128, 16, 512], ADT); u1_t = TL(16)
        with C.scope():
            u0 = C.alloc([128, 16, 512], ADT); u0_t = TL(16)
            S.dma("sp", u0, W["hy_u0"].rearrange("s p c -> p s c"), reads=[W["hy_u0_t"]], writes=u0_t)
            hyena_conv(C, W, 0, u0, u0_t, pnyq, pnyq_t, altcol, altrow, alt_t, out_tok=u1, out_tok_t=u1_t)
        if HY_STOP <= 4:
            out_proj(C, mixT, mixT_t, W["w_out0"], x_in, x_out)
            return
        hyena_filter(C, W, 1, h2T, h2_t, pnyq, pnyq_t, altcol, alt_t)
        hyena_conv(C, W, 1, u1, u1_t, pnyq, pnyq_t, altcol, altrow, alt_t, mixT=mixT, mixT_t=mixT_t)
        out_proj(C, mixT, mixT_t, W["w_out0"], x_in, x_out)


def host_consts():
    c = {}
    c["c_ident"] = np.eye(128, dtype=np.float32)
    sel = np.zeros((8, NE * 128), np.float32)
    for e in range(NE):
        sel[e, e * 128:(e + 1) * 128] = 1.0
    c["c_sel"] = sel
    c["c_iota"] = np.arange(CAP, dtype=np.float32)
    c["c_slotcol"] = (np.arange(128, dtype=np.float32)[:, None] + 128.0 * np.arange(NSL, dtype=np.float32)[None, :]).astype(np.float32)
    c["c_tri"] = np.triu(np.ones((128, 128), np.float32), k=1)
    i = np.arange(128)[:, None]; n = np.arange(256)[None, :]
    rel = i - n + 64
    band = np.abs(rel) <= 64
    c["c_negmask"] = np.where(band, 0.0, NEG).astype(np.float32)
    oh = np.zeros((3, 32, 128, 256), np.float32)
    buckets = []
    for g, (win, r) in enumerate(GROUPS):
        bk = _rel_bucket(rel * r)
        used = []
        for b in range(32):
            m = band & (bk == b)
            if m.any():
                oh[g, b] = m
                used.append(b)
        buckets.append(used)
    c["c_oh"] = oh
    c["_buckets"] = buckets
    import ml_dtypes
    L = TOK
    t = np.linspace(0.0, 1.0, L, dtype=np.float32)[:, None]
    f = np.linspace(1e-4, 15.0, 16, dtype=np.float32)[None]
    ang = (np.float32(2.0 * math.pi / L) * np.arange(L, dtype=np.float32)[:, None] * f).astype(np.float32)
    feats = np.concatenate([t, np.cos(ang), -np.sin(ang)], axis=-1).astype(np.float32)
    c["c_featsT"] = np.ascontiguousarray(feats.T)
    deltas = np.abs(np.linspace(math.log(1e-2) / 1.5, math.log(1e-2) / 0.3, 512, dtype=np.float32))
    c["c_decay"] = (np.exp(-t * deltas[None, :]) + 0.05).astype(np.float32)
    idx = np.outer(np.arange(L, dtype=np.int64), np.arange(L, dtype=np.int64)) % NFFT
    th = 2.0 * math.pi / NFFT
    for nm, fn in (("c_dftc", np.cos), ("c_dfts", np.sin)):
        full = fn(th * idx).astype(np.float32)
        til = full.reshape(16, 128, 8, 256).transpose(2, 1, 0, 3)
        c[nm] = np.ascontiguousarray(til).astype(ml_dtypes.bfloat16)
    sgn = np.where(np.arange(512) % 2 == 0, 1.0, -1.0).astype(np.float32)
    c["c_altcol"] = np.ascontiguousarray(np.repeat(sgn[:128, None], 128, axis=1))
    ar = np.zeros((128, 512), np.float32); ar[0] = sgn
    c["c_altrow"] = ar
    return c


def _rel_bucket(rel):
    half = 16
    exact = 8
    n = np.abs(rel)
    large = exact + (np.log(np.maximum(n, 1) / exact) / np.log(1024 / exact) * (half - exact)).astype(np.int32)
    large = np.minimum(large, half - 1)
    return (np.where(rel > 0, half, 0) + np.where(n < exact, n, large)).astype(np.int32)


def tile_gu(w):
    w = np.asarray(w, np.float32)
    E = w.shape[0]
    return np.ascontiguousarray(w.reshape(E, 8, 128, 11, 256).transpose(0, 3, 2, 1, 4))


def tile_d(w):
    w = np.asarray(w, np.float32)
    E = w.shape[0]
    wp = np.zeros((E, 24, 128, 2, 512), np.float32)
    wp[:, :22] = w.reshape(E, 22, 128, 2, 512)
    return np.ascontiguousarray(wp.reshape(E, 6, 4, 128, 2, 512).transpose(0, 4, 1, 3, 2, 5))


def pc(v):
    v = np.asarray(v, np.float32)
    return np.ascontiguousarray(v.reshape(-1, 128).T)


STAGE_INPUTS = {
    "ffn0": {"norm_ffn0": [128, 8], "ffn_w_gate": [1, 11, 128, 8, 256], "ffn_w_up": [1, 11, 128, 8, 256], "ffn_w_down": [1, 2, 6, 128, 4, 512]},
    "moe": {"norm_ffn1": [128, 8], "moe_router": [1024, NE], "moe_w_gate": [NE, 11, 128, 8, 256], "moe_w_up": [NE, 11, 128, 8, 256],
            "moe_w_down": [NE, 2, 6, 128, 4, 512], "c_sel": [8, NE * 128]},
    "moes": {"norm_ffn1": [128, 8], "norm_ffn1_row": [1024], "moe_router": [1024, NE], "moe_w_gate": [NE, 11, 128, 8, 256],
             "moe_w_up": [NE, 11, 128, 8, 256], "moe_w_down": [NE, 2, 6, 128, 4, 512], "c_iota": [CAP], "c_slotcol": [128, NSL], "c_tri": [128, 128]},
    "mix1": {"norm_mix1": [128, 8], "norm_mem1": [128, 8], "w_mem_kv1": [1024, 1024], "xq_norm1": [128, 1], "xk_norm1": [128, 1],
             "w_out1": [1024, 1024], "at_w_in": [1024, 5120], "at_q_norm": [128, 1], "at_k_norm": [128, 1], "rel_bias": [32, 12],
             "c_oh": [3, 32, 128, 256], "c_negmask": [128, 256], "mem": [MEM, D]},
    "mix0": {"norm_mix0": [128, 8], "norm_mem0": [128, 8], "w_mem_kv0": [1024, 1024], "xq_norm0": [128, 1], "xk_norm0": [128, 1],
             "w_out0": [1024, 1024], "hy_w_in": [1024, 2048], "hy_cwb": [128, 12, 4], "hy_fsm": [64, 3], "hy_filt_w1": [33, 64],
             "hy_filt_w2": [64, 64], "hy_filt_w3": [64, 2048], "hy_skip": [2, 512], "c_featsT": [33, TOK], "c_decay": [TOK, 512],
             "c_dftc": ([8, 128, 16, 256], "bf16"), "c_dfts": ([8, 128, 16, 256], "bf16"), "c_altcol": [128, 128], "c_altrow": [128, 512],
             "mem": [MEM, D]},
}
SCRATCH = {"hy_pq": [2, 2, 16, 128, 512], "hy_c1": [16, 128, 512], "hy_u0": [16, 128, 512], "hy_c2T": [4, 128, TOK]}


def build(stages):
    nc = bass.Bass("TRN2", target_bir_lowering=False)
    W = {}

    def ext(name, shape):
        dt = F32
        if isinstance(shape, tuple):
            shape, dt = shape[0], BF16
        if name not in W:
            W[name] = nc.dram_tensor(name, list(shape), dt, kind="ExternalInput").ap()
    if "mix0" in stages:
        for k, shp in SCRATCH.items():
            W[k] = nc.dram_tensor(k, list(shp), BF16, kind="Internal").ap()
    ext("c_ident", [128, 128])
    for st in stages:
        for k, shp in STAGE_INPUTS[st].items():
            ext(k, shp)
    xs = []
    for i in range(len(stages) + 1):
        if i == 0:
            xs.append(nc.dram_tensor("x_in", [TOK, D], F32, kind="ExternalInput").ap())
        elif i == len(stages):
            xs.append(nc.dram_tensor("x_out", [TOK, D], F32, kind="ExternalOutput").ap())
        else:
            xs.append(nc.dram_tensor("x_mid%d" % i, [TOK, D], F32, kind="Internal").ap())
    W["_buckets"] = host_consts()["_buckets"]
    with contextlib.ExitStack() as stack:
        C = Ctx(nc, stack)
        load_consts(C, W)
        for i, st in enumerate(stages):
            if st == "ffn0":
                stage_ffn(C, xs[i], xs[i + 1], W["norm_ffn0"], W["ffn_w_gate"], W["ffn_w_up"], W["ffn_w_down"])
            elif st == "moe":
                stage_ffn(C, xs[i], xs[i + 1], W["norm_ffn1"], W["moe_w_gate"], W["moe_w_up"], W["moe_w_down"],
                          router=W["moe_router"], sel=W["c_sel"])
            elif st == "moes":
                stage_moe_sparse(C, xs[i], xs[i + 1], W["norm_ffn1"], W["norm_ffn1_row"], W["moe_w_gate"], W["moe_w_up"],
                                 W["moe_w_down"], W["moe_router"], W)
            elif st == "mix1":
                stage_mix1(C, xs[i], xs[i + 1], W["mem"], W)
            elif st == "mix0":
                stage_mix0(C, xs[i], xs[i + 1], W["mem"], W)
            C.S.barrier()
        C.S.emit()
    return nc


def stage_host_inputs(stages, inputs, consts):
    m = {"c_ident": consts["c_ident"]}
    for st in stages:
        if st == "ffn0":
            m["norm_ffn0"] = pc(inputs["norm_ffn"][0])
            m["ffn_w_gate"] = tile_gu(inputs["ffn_w_gate"])
            m["ffn_w_up"] = tile_gu(inputs["ffn_w_up"])
            m["ffn_w_down"] = tile_d(inputs["ffn_w_down"])
        elif st == "moes":
            m["norm_ffn1"] = pc(inputs["norm_ffn"][1])
            m["norm_ffn1_row"] = np.ascontiguousarray(inputs["norm_ffn"][1], dtype=np.float32)
            m["moe_router"] = np.ascontiguousarray(inputs["moe_router"][0])
            m["moe_w_gate"] = tile_gu(inputs["moe_w_gate"][0])
            m["moe_w_up"] = tile_gu(inputs["moe_w_up"][0])
            m["moe_w_down"] = tile_d(inputs["moe_w_down"][0])
            for k in ("c_iota", "c_slotcol", "c_tri"):
                m[k] = consts[k]
        elif st == "moe":
            m["norm_ffn1"] = pc(inputs["norm_ffn"][1])
            m["moe_router"] = np.ascontiguousarray(inputs["moe_router"][0])
            m["moe_w_gate"] = tile_gu(inputs["moe_w_gate"][0])
            m["moe_w_up"] = tile_gu(inputs["moe_w_up"][0])
            m["moe_w_down"] = tile_d(inputs["moe_w_down"][0])
            m["c_sel"] = consts["c_sel"]
        elif st == "mix1":
            m["norm_mix1"] = pc(inputs["norm_mix"][1]); m["norm_mem1"] = pc(inputs["norm_mem"][1])
            m["w_mem_kv1"] = np.ascontiguousarray(inputs["w_mem_kv"][1]); m["w_out1"] = np.ascontiguousarray(inputs["w_out"][1])
            m["xq_norm1"] = pc(inputs["xq_norm"][1]); m["xk_norm1"] = pc(inputs["xk_norm"][1])
            m["at_w_in"] = np.ascontiguousarray(inputs["at_w_in"][0])
            m["at_q_norm"] = pc(inputs["at_q_norm"][0]); m["at_k_norm"] = pc(inputs["at_k_norm"][0])
            m["rel_bias"] = np.ascontiguousarray(inputs["rel_bias"], dtype=np.float32)
            m["c_oh"] = consts["c_oh"]; m["c_negmask"] = consts["c_negmask"]
        elif st == "mix0":
            m["norm_mix0"] = pc(inputs["norm_mix"][0]); m["norm_mem0"] = pc(inputs["norm_mem"][0])
            m["w_mem_kv0"] = np.ascontiguousarray(inputs["w_mem_kv"][0]); m["w_out0"] = np.ascontiguousarray(inputs["w_out"][0])
            m["xq_norm0"] = pc(inputs["xq_norm"][0]); m["xk_norm0"] = pc(inputs["xk_norm"][0])
            m["hy_w_in"] = np.ascontiguousarray(inputs["hy_w_in"][0])
            cw = np.asarray(inputs["hy_conv_w"][0], np.float32); cb = np.asarray(inputs["hy_conv_b"][0], np.float32)
            cwb = np.concatenate([cw, cb[None, :]], axis=0)
            m["hy_cwb"] = np.ascontiguousarray(cwb.reshape(4, 12, 128).transpose(2, 1, 0))
            m["hy_fsm"] = np.ascontiguousarray(np.stack([inputs["hy_sin_freq"][0], inputs["hy_filt_b1"][0], inputs["hy_filt_b2"][0]], axis=1), dtype=np.float32)
            m["hy_filt_w1"] = np.ascontiguousarray(inputs["hy_filt_w1"][0]); m["hy_filt_w2"] = np.ascontiguousarray(inputs["hy_filt_w2"][0])
            m["hy_filt_w3"] = np.ascontiguousarray(inputs["hy_filt_w3"][0]); m["hy_skip"] = np.ascontiguousarray(inputs["hy_skip"][0])
            for k in ("c_featsT", "c_decay", "c_dftc", "c_dfts", "c_altcol", "c_altrow"):
                m[k] = consts[k]
    return m


def run_stages(stages, x_list, inputs, core_ids=None, mem_list=None):
    consts = host_consts()
    nc = build(stages)
    wm = stage_host_inputs(stages, inputs, consts)
    n = len(x_list)
    in_maps = []
    for i in range(n):
        d = dict(wm)
        d["x_in"] = np.ascontiguousarray(x_list[i], dtype=np.float32)
        if mem_list is not None and any(st in ("mix0", "mix1") for st in stages):
            d["mem"] = np.ascontiguousarray(mem_list[i], dtype=np.float32)
        in_maps.append(d)
    res = run_bass_kernel_spmd(nc, in_maps, core_ids=list(range(n)) if core_ids is None else core_ids)
    return [r["x_out"] for r in res.results]


ALL_STAGES = ["mix0", "ffn0", "mix1", "moes"]


def kernel(**inputs):
    inputs = {k: np.asarray(v) for k, v in inputs.items()}
    B = inputs["x"].shape[0]
    outs = run_stages(ALL_STAGES, [inputs["x"][b] for b in range(B)], inputs,
                      mem_list=[inputs["mem"][b] for b in range(B)])
    return np.stack(outs, axis=0).astype(np.float32)
```

```python
import contextlib
import math
import os
HY_STOP = int(os.environ.get('HY_STOP', '9'))
HY_F = int(os.environ.get('HY_F', '9'))
import numpy as np
import concourse.bass as bass
import concourse.mybir as mybir
from concourse.bass_utils import run_bass_kernel_spmd

F32 = mybir.dt.float32
BF16 = mybir.dt.bfloat16
AF = mybir.ActivationFunctionType
ALU = mybir.AluOpType
AX = mybir.AxisListType

TOK = 2048
D = 1024
DC = 8
NT = 16
MEM = 256
DFF = 2816
NFF = 22
NE = 8
EPS = 1e-6
ADT = BF16
SAME_ENGINE_SYNC = True
ARENA_BYTES = 174 * 1024


class T:
    __slots__ = ("w", "r")

    def __init__(self):
        self.w = None
        self.r = {}


def TL(n):
    return [T() for _ in range(n)]


class Sched:
    ENGS = ("pe", "act", "dve", "pool", "sp")
    NDMA = {"sp": 8, "pool": 4, "act": 4}

    def __init__(self, nc, stack):
        self.nc = nc
        self.streams = {e: [] for e in self.ENGS}
        self.cnt = {e: 0 for e in self.ENGS}
        self.sems = {}
        for e in ("pe", "act", "dve", "pool"):
            self.sems[e] = stack.enter_context(nc.semaphore("c_" + e))
        self.dma_i = {q: 0 for q in self.NDMA}
        for q, k in self.NDMA.items():
            for s in range(k):
                self.sems[("dma", q, s)] = stack.enter_context(nc.semaphore("d_%s_%d" % (q, s)))
        self.waited = {}
        self.final = {}

    def _collect(self, eng, reads, writes, skip_self):
        deps = {}

        def add(ev):
            if ev is None:
                return
            k, v = ev
            if deps.get(k, 0) < v:
                deps[k] = v
        for t in reads:
            add(t.w)
        for t in writes:
            add(t.w)
            for k, v in t.r.items():
                add((k, v))
        waits = []
        for k, v in deps.items():
            if k == eng and (skip_self or not SAME_ENGINE_SYNC):
                continue
            if self.waited.get((eng, k), 0) >= v:
                continue
            self.waited[(eng, k)] = v
            waits.append((k, v))
        return waits

    def _mark(self, ev, reads, writes):
        k, v = ev
        for t in reads:
            t.r[k] = v
        for t in writes:
            t.w = ev
            t.r = {}
        if self.final.get(k, 0) < v:
            self.final[k] = v

    def op(self, eng, fn, reads=(), writes=(), skip_self=None):
        if skip_self is None:
            skip_self = (eng == "pe")
        waits = self._collect(eng, reads, writes, skip_self)
        self.cnt[eng] += 1
        ev = (eng, self.cnt[eng])
        self.streams[eng].append((waits, fn, (eng, 1)))
        self._mark(ev, reads, writes)
        return ev

    def dma(self, q, out, in_, reads=(), writes=(), **kw):
        i = self.dma_i[q]
        K = self.NDMA[q]
        slot = i % K
        key = ("dma", q, slot)
        waits = self._collect(q, reads, writes, False)
        if i >= K:
            prev = 16 * (i // K)
            if self.waited.get((q, key), 0) < prev:
                self.waited[(q, key)] = prev
                waits.append((key, prev))
        self.dma_i[q] = i + 1
        ev = (key, 16 * (i // K + 1))

        def fn(e, out=out, in_=in_, kw=kw):
            return e.dma_start(out=out, in_=in_, **kw)
        self.streams[q].append((waits, fn, (key, 16)))
        self._mark(ev, reads, writes)
        return ev

    def barrier(self):
        for e in self.ENGS:
            waits = []
            for k, v in self.final.items():
                if k == e:
                    continue
                if self.waited.get((e, k), 0) >= v:
                    continue
                self.waited[(e, k)] = v
                waits.append((k, v))
            if waits:
                self.streams[e].append((waits, None, None))

    def emit(self):
        nc = self.nc
        self.barrier()
        sems = self.sems
        streams = self.streams

        def run(e, name):
            for waits, fn, inc in streams[name]:
                for k, v in waits:
                    e.wait_ge(sems[k], v)
                if fn is not None:
                    inst = fn(e)
                    inst.then_inc(sems[inc[0]], inc[1])

        with nc.Block() as block:
            @block.tensor
            def _(e):
                run(e, "pe")

            @block.scalar
            def _(e):
                run(e, "act")

            @block.vector
            def _(e):
                run(e, "dve")

            @block.gpsimd
            def _(e):
                run(e, "pool")

            @block.sync
            def _(e):
                run(e, "sp")


def dtsize(dt):
    return mybir.dt.size(dt)


class Ctx:
    def __init__(self, nc, stack):
        self.nc = nc
        self.S = Sched(nc, stack)
        self.arena = stack.enter_context(nc.sbuf_tensor("arena", [128, ARENA_BYTES // 4], F32))[:, :]
        self.off = 0
        self.banks = [stack.enter_context(nc.psum_tensor("bank%d" % i, [128, 512], F32))[:, :] for i in range(8)]
        self.bank_t = TL(8)
        self.bank_i = 0
        self.reserved = set()
        self.drams = {}

    def alloc(self, shape, dt=F32):
        per = int(np.prod(shape[1:])) * dtsize(dt)
        per = (per + 31) // 32 * 32
        assert self.off + per <= ARENA_BYTES, ("SBUF arena overflow", self.off, per)
        v = self.arena[0:shape[0], self.off // 4:(self.off + per) // 4]
        self.off += per
        self.peak = max(getattr(self, 'peak', 0), self.off)
        if dt != F32:
            v = v.bitcast(dt)
        n = int(np.prod(shape[1:]))
        v = v[:, 0:n]
        if len(shape) == 3:
            v = v.rearrange("p (a b) -> p a b", a=shape[1])
        elif len(shape) == 4:
            v = v.rearrange("p (a b c) -> p a b c", a=shape[1], b=shape[2])
        return v

    @contextlib.contextmanager
    def scope(self):
        save = self.off
        yield
        self.S.barrier()
        self.off = save

    def psum(self):
        while True:
            i = self.bank_i
            self.bank_i = (i + 1) % 8
            if i not in self.reserved:
                return self.banks[i], self.bank_t[i]

    def reserve(self):
        i = self.bank_i
        self.bank_i = (i + 1) % 8
        self.reserved.add(i)
        return i

    def release(self, i):
        self.reserved.discard(i)

    def mm(self, out, pairs, reads, writes, **kw):
        pairs = list(pairs)

        def fn(e, out=out, pairs=pairs, kw=kw):
            n = len(pairs)
            inst = None
            for i, (l, r) in enumerate(pairs):
                inst = e.matmul(out, l, r, start=(i == 0), stop=(i == n - 1), **kw)
            return inst
        return self.S.op("pe", fn, reads=reads, writes=writes)


def load_consts(C, W):
    S = C.S
    C.ident_f = C.alloc([128, 128], F32)
    C.ident_b = C.alloc([128, 128], BF16)
    C.ones_f = C.alloc([128, 128], F32)
    C.ones_b = C.alloc([128, 128], BF16)
    C.ct = T()
    S.dma("sp", C.ident_f, W["c_ident"], writes=[C.ct])
    S.op("dve", lambda e: e.tensor_copy(C.ident_b, C.ident_f), reads=[C.ct], writes=[C.ct])
    S.op("dve", lambda e: e.memset(C.ones_f, 1.0), writes=[C.ct])
    S.op("dve", lambda e: e.memset(C.ones_b, 1.0), writes=[C.ct])


def rstd_from_ss(C, out, ss, n, reads, writes, tmp):
    S = C.S
    S.op("act", lambda e: e.activation(tmp, ss, AF.Ln, bias=EPS, scale=1.0 / n), reads=list(reads), writes=writes)
    S.op("act", lambda e: e.activation(out, tmp, AF.Exp, scale=-0.5), reads=writes, writes=writes)


def norm_T(C, x_ap, ntok, g_ap, hT, hT_t, hook=None, copy_to=None, copy_t=None, hook_xn=False):
    S = C.S
    nt = ntok // 128
    with C.scope():
        g_sb = C.alloc([128, 8]); g_t = T()
        S.dma("sp", g_sb, g_ap, writes=[g_t])
        xt = [C.alloc([128, 1024]) for _ in range(2)]; xt_t = TL(2)
        xn = [C.alloc([128, 1024]) for _ in range(2)]; xn_t = TL(2)
        junk = C.alloc([128, 1024], BF16); junk_t = T()
        st = C.alloc([128, 3 * nt]); st_t = TL(nt)
        h32 = None
        if hook is not None:
            h32 = [C.alloc([128, 8, 128]) for _ in range(2)]; h32_t = TL(2)
        for t in range(nt):
            s = t % 2
            S.dma("sp", xt[s], x_ap[t * 128:(t + 1) * 128, :], writes=[xt_t[s]])
            if copy_to is not None:
                S.dma("sp", copy_to[t * 128:(t + 1) * 128, :], xt[s], reads=[xt_t[s]], writes=[copy_t[t]])
            ss = st[:, 3 * t:3 * t + 1]; ln = st[:, 3 * t + 1:3 * t + 2]; rs = st[:, 3 * t + 2:3 * t + 3]
            S.op("act", lambda e, s=s, ss=ss: e.activation(junk, xt[s], AF.Square, accum_out=ss),
                 reads=[xt_t[s]], writes=[junk_t, st_t[t]])
            rstd_from_ss(C, rs, ss, 1024.0, [st_t[t]], [st_t[t]], ln)
            S.op("dve", lambda e, s=s, rs=rs: e.tensor_scalar(xn[s], xt[s], rs, None, ALU.mult),
                 reads=[xt_t[s], st_t[t]], writes=[xn_t[s]])
            for half in range(2):
                ps, pt = C.psum()

                def tr(e, s=s, half=half, ps=ps):
                    inst = None
                    for j in range(4):
                        c = half * 4 + j
                        inst = e.transpose(ps[:, j * 128:(j + 1) * 128], xn[s][:, c * 128:(c + 1) * 128], C.ident_f)
                    return inst
                S.op("pe", tr, reads=[xn_t[s], C.ct], writes=[pt])
                gb = g_sb[:, half * 4:half * 4 + 4].unsqueeze(2).to_broadcast([128, 4, 128])
                psv = ps[:].rearrange("p (a b) -> p a b", a=4)
                if hook is None:
                    S.op("dve", lambda e, t=t, half=half, psv=psv, gb=gb: e.tensor_tensor(
                        hT[:, half * 4:half * 4 + 4, t * 128:(t + 1) * 128], psv, gb, ALU.mult),
                        reads=[pt, g_t], writes=[hT_t[t]])
                else:
                    S.op("dve", lambda e, s=s, half=half, psv=psv, gb=gb: e.tensor_tensor(
                        h32[s][:, half * 4:half * 4 + 4, :], psv, gb, ALU.mult),
                        reads=[pt, g_t], writes=[h32_t[s]])
                    if hT is not None:
                        S.op("act", lambda e, s=s, t=t, half=half: e.copy(
                            hT[:, half * 4:half * 4 + 4, t * 128:(t + 1) * 128], h32[s][:, half * 4:half * 4 + 4, :]),
                            reads=[h32_t[s]], writes=[hT_t[t]])
            if hook is not None:
                if hook_xn:
                    hook(t, h32[s], h32_t[s], xn[s], xn_t[s])
                else:
                    hook(t, h32[s], h32_t[s])


class WStream:
    def __init__(self, C, nstage=3, nslot=4, elems=2048):
        self.C = C
        self.stage = [C.alloc([128, elems], F32) for _ in range(nstage)]
        self.stage_t = TL(nstage)
        self.slot = [C.alloc([128, elems], BF16) for _ in range(nslot)]
        self.slot_t = TL(nslot)
        self.i = 0
        self.j = 0
        self.pending = []

    def issue(self, src_ap, shape, cast_eng):
        C = self.C; S = C.S
        si = self.i % len(self.stage); self.i += 1
        sj = self.j % len(self.slot); self.j += 1
        n = int(np.prod(shape[1:]))
        stg = self.stage[si][:, 0:n]
        dst = self.slot[sj][:, 0:n]
        if len(shape) == 3:
            stg3 = stg.rearrange("p (a b) -> p a b", a=shape[1])
            dst3 = dst.rearrange("p (a b) -> p a b", a=shape[1])
        else:
            stg3, dst3 = stg, dst
        S.dma("sp", stg3, src_ap, writes=[self.stage_t[si]])
        if cast_eng == "act":
            S.op("act", lambda e: e.copy(dst, stg), reads=[self.stage_t[si]], writes=[self.slot_t[sj]])
        elif cast_eng == "dve":
            S.op("dve", lambda e: e.tensor_copy(dst, stg), reads=[self.stage_t[si]], writes=[self.slot_t[sj]])
        else:
            S.op("pool", lambda e: e.tensor_copy(dst, stg), reads=[self.stage_t[si]], writes=[self.slot_t[sj]])
        return dst3, self.slot_t[sj]


def wgu_slab(Wt, e, s_):
    return Wt[e, s_]


def wd_slab(Wt, e, half, s4, nf):
    return Wt[e, half, s4][:, 0:nf, :]


def wview(w_ap, c0, nc_, n0, nn):
    return w_ap.rearrange("(c p) n -> p c n", p=128)[:, c0:c0 + nc_, n0:n0 + nn]


def stage_ffn(C, x_in, x_out, g_ap, Wg, Wu, Wd, router=None, sel=None):
    S = C.S
    E = Wg.shape[0]
    moe = router is not None
    with C.scope():
        hT = C.alloc([128, 8, TOK], ADT); hT_t = TL(NT)
        GT = None
        if moe:
            GT = C.alloc([8, TOK], F32); GT_t = TL(NT)
            r_sb = C.alloc([128, 8, 8], F32); r_t = T()
            S.dma("sp", r_sb, router.rearrange("(c p) e -> p c e", p=128), writes=[r_t])
            sel_sb = C.alloc([8, NE * 128], F32); sel_t = T()
            S.dma("sp", sel_sb, sel, writes=[sel_t])
            gsm = [C.alloc([128, 64], F32) for _ in range(2)]; gsm_t = TL(2)

            def hook(t, h32, h32_t):
                s = t % 2
                ps, pt = C.psum()
                C.mm(ps[:, 0:8], [(h32[:, c, :], r_sb[:, c, :]) for c in range(8)], [h32_t, r_t], [pt])
                g = gsm[s]
                lg = g[:, 0:8]; top = g[:, 8:16]; nv1 = g[:, 16:17]; msk = g[:, 24:32]; ex = g[:, 32:40]
                mex = g[:, 40:48]; den = g[:, 48:49]; rden = g[:, 49:50]; G = g[:, 56:64]
                S.op("dve", lambda e: e.tensor_copy(lg, ps[:, 0:8]), reads=[pt], writes=[gsm_t[s]])
                S.op("dve", lambda e: e.max(top, lg), reads=[gsm_t[s]], writes=[gsm_t[s]])
                S.op("dve", lambda e: e.tensor_scalar(nv1, top[:, 0:1], -1.0, None, ALU.mult), reads=[gsm_t[s]], writes=[gsm_t[s]])
                S.op("dve", lambda e: e.tensor_scalar(msk, lg, top[:, 1:2], None, ALU.is_ge), reads=[gsm_t[s]], writes=[gsm_t[s]])
                S.op("act", lambda e: e.activation(ex, lg, AF.Exp, bias=nv1, scale=1.0), reads=[gsm_t[s]], writes=[gsm_t[s]])
                S.op("dve", lambda e: e.tensor_tensor(mex, msk, ex, ALU.mult), reads=[gsm_t[s]], writes=[gsm_t[s]])
                S.op("dve", lambda e: e.reduce_sum(den, mex, AX.X), reads=[gsm_t[s]], writes=[gsm_t[s]])
                S.op("dve", lambda e: e.reciprocal(rden, den), reads=[gsm_t[s]], writes=[gsm_t[s]])
                S.op("dve", lambda e: e.tensor_scalar(G, mex, rden, None, ALU.mult), reads=[gsm_t[s]], writes=[gsm_t[s]])
                ps2, pt2 = C.psum()
                S.op("pe", lambda e: e.transpose(ps2[0:8, 0:128], G, C.ident_f), reads=[gsm_t[s], C.ct], writes=[pt2])
                S.op("dve", lambda e: e.tensor_copy(GT[:, t * 128:(t + 1) * 128], ps2[0:8, 0:128]), reads=[pt2], writes=[GT_t[t]])
            norm_T(C, x_in, TOK, g_ap, hT, hT_t, hook=hook)
        else:
            norm_T(C, x_in, TOK, g_ap, hT, hT_t)

        TG = 1024
        NTG = TOK // TG
        aT = C.alloc([128, NFF, TG], ADT); aT_t = TL(NFF)
        yacc = C.alloc([128, 8, 1024], F32); yacc_t = TL(16)
        sg = [C.alloc([128, 512], F32) for _ in range(2)]; sg_t = TL(2)
        grep = C.alloc([128, TG], ADT); grep_t = T()
        ws = WStream(C, nstage=3, nslot=4)
        xin_v = x_in.rearrange("(t p) d -> p t d", p=128)
        xout_v = x_out.rearrange("(t p) d -> p t d", p=128)
        k = 0
        for tg in range(NTG):
            ya = yacc; ya_t = yacc_t
            S.dma("sp", ya, xin_v[:, tg * 8:(tg + 1) * 8, :], writes=ya_t)
            for ex_i in range(E):
                if moe:
                    for blk in range(2):
                        tok = slice(tg * TG + blk * 512, tg * TG + (blk + 1) * 512)
                        ps, pt = C.psum()
                        C.mm(ps, [(sel_sb[:, ex_i * 128:(ex_i + 1) * 128], GT[:, tok])], [sel_t] + GT_t[tg * 8:tg * 8 + 8], [pt])
                        S.op("act", lambda e, ps=ps, blk=blk: e.copy(grep[:, blk * 512:(blk + 1) * 512], ps), reads=[pt], writes=[grep_t])
                for s in range(11):
                    wg_b, wg_t = ws.issue(wgu_slab(Wg, ex_i, s), [128, 8, 256], "pool")
                    wu_b, wu_t = ws.issue(wgu_slab(Wu, ex_i, s), [128, 8, 256], "pool")
                    for j in range(2):
                        ff = 2 * s + j
                        for blk in range(2):
                            tok = slice(tg * TG + blk * 512, tg * TG + (blk + 1) * 512)
                            lt = slice(blk * 512, (blk + 1) * 512)
                            hts = hT_t[tg * 8 + blk * 4:tg * 8 + blk * 4 + 4]
                            pg, pgt = C.psum()
                            C.mm(pg, [(wg_b[:, c, j * 128:(j + 1) * 128], hT[:, c, tok]) for c in range(8)], [wg_t] + hts, [pgt])
                            pu, put = C.psum()
                            C.mm(pu, [(wu_b[:, c, j * 128:(j + 1) * 128], hT[:, c, tok]) for c in range(8)], [wu_t] + hts, [put])
                            sgi = k % 2; k += 1
                            S.op("act", lambda e, sgi=sgi, pg=pg: e.activation(sg[sgi], pg, AF.Silu), reads=[pgt], writes=[sg_t[sgi]])
                            if moe:
                                S.op("dve", lambda e, sgi=sgi, pu=pu: e.tensor_tensor(sg[sgi], sg[sgi], pu, ALU.mult),
                                     reads=[put, sg_t[sgi]], writes=[sg_t[sgi]])
                                S.op("pool", lambda e, sgi=sgi, ff=ff, lt=lt: e.tensor_tensor(aT[:, ff, lt], sg[sgi], grep[:, lt], ALU.mult),
                                     reads=[sg_t[sgi], grep_t], writes=[aT_t[ff]])
                            else:
                                S.op("dve", lambda e, sgi=sgi, pu=pu, ff=ff, lt=lt: e.tensor_tensor(aT[:, ff, lt], sg[sgi], pu, ALU.mult),
                                     reads=[put, sg_t[sgi]], writes=[aT_t[ff]])
                for half in range(2):
                    for s4 in range(6):
                        nf = 4 if s4 < 5 else 2
                        wd_b, wd_t = ws.issue(wd_slab(Wd, ex_i, half, s4, nf), [128, nf, 512], "act")

                        def dn(e, s4=s4, nf=nf, wd_b=wd_b):
                            inst = None
                            for j in range(nf):
                                ff = 4 * s4 + j
                                for tt in range(8):
                                    inst = e.matmul(C.banks[tt], aT[:, ff, tt * 128:(tt + 1) * 128], wd_b[:, j, :],
                                                    start=(ff == 0), stop=(ff == NFF - 1))
                            return inst
                        S.op("pe", dn, reads=[wd_t] + aT_t[4 * s4:4 * s4 + nf], writes=C.bank_t)
                    for tt in range(8):
                        b = tt * 2 + half
                        S.op("dve", lambda e, tt=tt, half=half: e.tensor_tensor(
                            ya[:, tt, half * 512:(half + 1) * 512], ya[:, tt, half * 512:(half + 1) * 512], C.banks[tt], ALU.add),
                            reads=[C.bank_t[tt], ya_t[b]], writes=[ya_t[b]])
            S.dma("sp", xout_v[:, tg * 8:(tg + 1) * 8, :], ya, reads=ya_t)


CAP = 768
NSL = CAP // 128
NBH = CAP // 2


def stage_moe_sparse(C, x_in, x_out, g_ap, grow_ap, Wg, Wu, Wd, router, W):
    S = C.S
    with C.scope():
        htm = C.alloc([128, NT, 1024], ADT); htm_t = TL(NT)
        pos_sb = C.alloc([128, NT, 8], F32); msk_sb = C.alloc([128, NT, 8], F32); G_sb = C.alloc([128, NT, 8], F32); rt_t = TL(NT)
        msum = C.alloc([128, 8], F32); msum_t = T()
        S.op("pool", lambda e: e.memset(msum, 0.0), writes=[msum_t])
        iota_row = C.alloc([128, CAP], F32); slotcol = C.alloc([128, NSL], F32); tri = C.alloc([128, 128], F32); cst_t = T()
        S.dma("sp", iota_row, W["c_iota"].partition_broadcast(128), writes=[cst_t])
        S.dma("sp", slotcol, W["c_slotcol"], writes=[cst_t])
        S.dma("sp", tri, W["c_tri"], writes=[cst_t])
        xo_t = TL(NT)
        with C.scope():
            r_sb = C.alloc([128, 8, 8], F32); r_t = T()
            S.dma("sp", r_sb, router.rearrange("(c p) e -> p c e", p=128), writes=[r_t])
            grow = C.alloc([128, 1024], F32); grow_t = T()
            S.dma("sp", grow, grow_ap.partition_broadcast(128), writes=[grow_t])
            gsm = [C.alloc([128, 64], F32) for _ in range(2)]; gsm_t = TL(2)

            def hook(t, h32, h32_t, xn, xn_t):
                s = t % 2
                S.op("pool", lambda e: e.tensor_tensor(htm[:, t, :], xn, grow, ALU.mult), reads=[xn_t, grow_t], writes=[htm_t[t]])
                ps, pt = C.psum()
                C.mm(ps[:, 0:8], [(h32[:, c, :], r_sb[:, c, :]) for c in range(8)], [h32_t, r_t], [pt])
                g = gsm[s]
                lg = g[:, 0:8]; top = g[:, 8:16]; nv1 = g[:, 16:17]; ex = g[:, 32:40]
                mex = g[:, 40:48]; den = g[:, 48:49]; rden = g[:, 49:50]
                msk = msk_sb[:, t, :]
                S.op("dve", lambda e: e.tensor_copy(lg, ps[:, 0:8]), reads=[pt], writes=[gsm_t[s]])
                S.op("dve", lambda e: e.max(top, lg), reads=[gsm_t[s]], writes=[gsm_t[s]])
                S.op("dve", lambda e: e.tensor_scalar(nv1, top[:, 0:1], -1.0, None, ALU.mult), reads=[gsm_t[s]], writes=[gsm_t[s]])
                S.op("dve", lambda e: e.tensor_scalar(msk, lg, top[:, 1:2], None, ALU.is_ge), reads=[gsm_t[s]], writes=[rt_t[t]])
                S.op("act", lambda e: e.activation(ex, lg, AF.Exp, bias=nv1, scale=1.0), reads=[gsm_t[s]], writes=[gsm_t[s]])
                S.op("dve", lambda e: e.tensor_tensor(mex, msk, ex, ALU.mult), reads=[gsm_t[s], rt_t[t]], writes=[gsm_t[s]])
                S.op("dve", lambda e: e.reduce_sum(den, mex, AX.X), reads=[gsm_t[s]], writes=[gsm_t[s]])
                S.op("dve", lambda e: e.reciprocal(rden, den), reads=[gsm_t[s]], writes=[gsm_t[s]])
                S.op("dve", lambda e: e.tensor_scalar(G_sb[:, t, :], mex, rden, None, ALU.mult), reads=[gsm_t[s]], writes=[rt_t[t]])
                ps2, pt2 = C.psum()
                C.mm(ps2[:, 0:8], [(tri, msk), (C.ones_f, msum)], [rt_t[t], msum_t, cst_t, C.ct], [pt2])
                S.op("dve", lambda e: e.tensor_copy(pos_sb[:, t, :], ps2[:, 0:8]), reads=[pt2], writes=[rt_t[t]])
                S.op("dve", lambda e: e.tensor_tensor(msum, msum, msk, ALU.add), reads=[rt_t[t], msum_t], writes=[msum_t])
            norm_T(C, x_in, TOK, g_ap, None, None, hook=hook, copy_to=x_out, copy_t=xo_t, hook_xn=True)

        ws = WStream(C, nstage=3, nslot=3)
        Sel = C.alloc([128, NT, NBH], ADT); Sel_t = TL(NT)
        hgT = C.alloc([128, 8, CAP], ADT); hg_t = TL(2)
        aT = C.alloc([128, NFF, CAP], ADT); aT_t = TL(NFF)
        ye = C.alloc([128, NSL, 1024], ADT); ye_t = TL(NSL)
        sg = [C.alloc([128, NBH], F32) for _ in range(2)]; sg_t = TL(2)
        dgp = [C.alloc([128, 128], F32) for _ in range(2)]; dgg = [C.alloc([128, 128], F32) for _ in range(2)]; dg_t = TL(2)
        grep = [C.alloc([128, 256], F32) for _ in range(2)]; grep_t = TL(2)
        SelGT = [C.alloc([128, NSL, 256], ADT) for _ in range(2)]; SelGT_t = TL(2)
        ystg = [C.alloc([128, 2, 1024], F32) for _ in range(2)]; ystg_t = TL(2)
        xout_v = x_out.rearrange("(t p) d -> p t d", p=128)
        ksg = 0; kd = 0; ktp = 0
        pre_issued = []
        for ex_i in range(NE):
            for nb in range(2):
                for t in range(NT):
                    S.op("dve", lambda e, t=t, nb=nb, ex_i=ex_i: e.tensor_scalar(
                        Sel[:, t, :], iota_row[:, nb * NBH:(nb + 1) * NBH], pos_sb[:, t, ex_i:ex_i + 1], msk_sb[:, t, ex_i:ex_i + 1],
                        ALU.is_equal, ALU.mult), reads=[rt_t[t], cst_t], writes=[Sel_t[t]])
                for dc in range(8):
                    ps, pt = C.psum()
                    C.mm(ps[:, 0:NBH], [(htm[:, t, dc * 128:(dc + 1) * 128], Sel[:, t, :]) for t in range(NT)], htm_t + Sel_t, [pt])
                    S.op("act", lambda e, ps=ps, dc=dc, nb=nb: e.copy(hgT[:, dc, nb * NBH:(nb + 1) * NBH], ps[:, 0:NBH]),
                         reads=[pt], writes=[hg_t[nb]])
            handles = pre_issued
            pre_issued = []
            for s in range(11):
                if handles:
                    (wg_b, wg_t), (wu_b, wu_t) = handles[0], handles[1]
                    handles = handles[2:]
                else:
                    wg_b, wg_t = ws.issue(wgu_slab(Wg, ex_i, s), [128, 8, 256], "pool")
                    wu_b, wu_t = ws.issue(wgu_slab(Wu, ex_i, s), [128, 8, 256], "pool")
                for j in range(2):
                    ff = 2 * s + j
                    for nb in range(2):
                        sl = slice(nb * NBH, (nb + 1) * NBH)
                        pg, pgt = C.psum()
                        C.mm(pg[:, 0:NBH], [(wg_b[:, c, j * 128:(j + 1) * 128], hgT[:, c, sl]) for c in range(8)], [wg_t, hg_t[nb]], [pgt])
                        pu, put = C.psum()
                        C.mm(pu[:, 0:NBH], [(wu_b[:, c, j * 128:(j + 1) * 128], hgT[:, c, sl]) for c in range(8)], [wu_t, hg_t[nb]], [put])
                        sgi = ksg % 2; ksg += 1
                        S.op("act", lambda e, sgi=sgi, pg=pg: e.activation(sg[sgi], pg[:, 0:NBH], AF.Silu), reads=[pgt], writes=[sg_t[sgi]])
                        S.op("dve", lambda e, sgi=sgi, pu=pu, ff=ff, sl=sl: e.tensor_tensor(aT[:, ff, sl], sg[sgi], pu[:, 0:NBH], ALU.mult),
                             reads=[put, sg_t[sgi]], writes=[aT_t[ff]])
            for half in range(2):
                for s4 in range(6):
                    nf = 4 if s4 < 5 else 2
                    wd_b, wd_t = ws.issue(wd_slab(Wd, ex_i, half, s4, nf), [128, nf, 512], "act")

                    def dn(e, s4=s4, nf=nf, wd_b=wd_b):
                        inst = None
                        for j in range(nf):
                            ff = 4 * s4 + j
                            for st_ in range(NSL):
                                inst = e.matmul(C.banks[st_], aT[:, ff, st_ * 128:(st_ + 1) * 128], wd_b[:, j, :],
                                                start=(ff == 0), stop=(ff == NFF - 1))
                        return inst
                    S.op("pe", dn, reads=[wd_t] + aT_t[4 * s4:4 * s4 + nf], writes=C.bank_t[0:NSL])
                for st_ in range(NSL):
                    S.op("act", lambda e, st_=st_, half=half: e.copy(ye[:, st_, half * 512:(half + 1) * 512], C.banks[st_]),
                         reads=[C.bank_t[st_]], writes=[ye_t[st_]])
            if ex_i + 1 < NE:
                pre_issued = [ws.issue(wgu_slab(Wg, ex_i + 1, 0), [128, 8, 256], "pool"),
                              ws.issue(wgu_slab(Wu, ex_i + 1, 0), [128, 8, 256], "pool")]
            for tp in range(8):
                i = ktp % 2; ktp += 1
                pp, ppt = C.psum()
                pgp, pgpt = C.psum()
                for q in range(2):
                    t = 2 * tp + q
                    d = kd % 2; kd += 1
                    S.op("dve", lambda e, d=d, t=t, ex_i=ex_i: e.tensor_scalar(dgp[d], C.ident_f, pos_sb[:, t, ex_i:ex_i + 1], None, ALU.mult),
                         reads=[rt_t[t], C.ct], writes=[dg_t[d]])
                    S.op("dve", lambda e, d=d, t=t, ex_i=ex_i: e.tensor_scalar(dgg[d], C.ident_f, G_sb[:, t, ex_i:ex_i + 1], None, ALU.mult),
                         reads=[rt_t[t], C.ct, dg_t[d]], writes=[dg_t[d]])
                    C.mm(pp[:, q * 128:(q + 1) * 128], [(C.ones_f, dgp[d])], [dg_t[d], C.ct], [ppt])
                    C.mm(pgp[:, q * 128:(q + 1) * 128], [(C.ones_f, dgg[d])], [dg_t[d], C.ct], [pgpt])
                S.op("act", lambda e, i=i, pgp=pgp: e.copy(grep[i], pgp[:, 0:256]), reads=[pgpt], writes=[grep_t[i]])
                for j in range(NSL):
                    S.op("dve", lambda e, i=i, j=j, pp=pp: e.scalar_tensor_tensor(
                        SelGT[i][:, j, :], pp[:, 0:256], slotcol[:, j:j + 1], grep[i], ALU.is_equal, ALU.mult),
                        reads=[ppt, grep_t[i], cst_t], writes=[SelGT_t[i]])
                S.dma("sp", ystg[i], xout_v[:, 2 * tp:2 * tp + 2, :], reads=[xo_t[2 * tp], xo_t[2 * tp + 1]], writes=[ystg_t[i]])
                for q in range(2):
                    for half in range(2):
                        ps, pt = C.psum()
                        C.mm(ps, [(SelGT[i][:, j, q * 128:(q + 1) * 128], ye[:, j, half * 512:(half + 1) * 512]) for j in range(NSL)],
                             [SelGT_t[i]] + ye_t, [pt])
                        S.op("dve", lambda e, i=i, q=q, half=half, ps=ps: e.tensor_tensor(
                            ystg[i][:, q, half * 512:(half + 1) * 512], ystg[i][:, q, half * 512:(half + 1) * 512], ps, ALU.add),
                            reads=[pt, ystg_t[i]], writes=[ystg_t[i]])
                S.dma("sp", xout_v[:, 2 * tp:2 * tp + 2, :], ystg[i], reads=[ystg_t[i]], writes=[xo_t[2 * tp], xo_t[2 * tp + 1]])


SCALE = 128.0 ** -0.5
GROUPS = ((128, 1), (512, 4), (2048, 16))
NEG = -80.0


class HeadNorm:
    def __init__(self, C):
        self.C = C
        self.raw = [C.alloc([128, 512]) for _ in range(2)]; self.raw_t = TL(2)
        self.sq = [C.alloc([128, 512]) for _ in range(2)]; self.sq_t = TL(2)
        self.sqb = [C.alloc([128, 512], ADT) for _ in range(2)]; self.sqb_t = TL(2)
        self.rs = [C.alloc([128, 512]) for _ in range(2)]; self.rs_t = TL(2)
        self.k = 0

    def run(self, ps, pt, n, g_col, g_t, out_ap, out_writes):
        C = self.C; S = C.S
        i = self.k % 2; self.k += 1
        raw = self.raw[i][:, 0:n]; sq = self.sq[i][:, 0:n]; rs = self.rs[i][:, 0:n]
        S.op("dve", lambda e: e.tensor_copy(raw, ps[:, 0:n]), reads=[pt], writes=[self.raw_t[i]])
        sqb = self.sqb[i][:, 0:n]
        S.op("act", lambda e: e.activation(sqb, raw, AF.Square), reads=[self.raw_t[i]], writes=[self.sqb_t[i]])
        p2, p2t = C.psum()
        C.mm(p2[:, 0:n], [(C.ones_b, sqb)], [self.sqb_t[i], C.ct], [p2t])
        S.op("act", lambda e: e.activation(sq, p2[:, 0:n], AF.Ln, bias=EPS, scale=1.0 / 128), reads=[p2t], writes=[self.sq_t[i]])
        S.op("act", lambda e: e.activation(rs, sq, AF.Exp, scale=-0.5), reads=[self.sq_t[i]], writes=[self.rs_t[i]])
        S.op("dve", lambda e: e.scalar_tensor_tensor(out_ap, raw, g_col, rs, ALU.mult, ALU.mult),
             reads=[self.raw_t[i], self.rs_t[i], g_t], writes=out_writes)


def mem_kv(C, mem_ap, gmem_ap, wkv_ap, gk_col, gk_t, kT, v, kv_t):
    S = C.S
    with C.scope():
        mT = C.alloc([128, 8, MEM], ADT); mT_t = TL(2)
        norm_T(C, mem_ap, MEM, gmem_ap, mT, mT_t)
        ws = WStream(C, nstage=2, nslot=2)
        hn = HeadNorm(C)
        for sl in range(2):
            wb, wt = ws.issue(wview(wkv_ap, 0, 8, sl * 256, 256), [128, 8, 256], "pool")
            for j in range(2):
                h = sl * 2 + j
                ps, pt = C.psum()
                C.mm(ps[:, 0:MEM], [(wb[:, c, j * 128:(j + 1) * 128], mT[:, c, :]) for c in range(8)], [wt] + mT_t, [pt])
                hn.run(ps, pt, MEM, gk_col, gk_t, kT[:, h, :], [kv_t])
        for sl in range(2):
            wb, wt = ws.issue(wview(wkv_ap, 0, 8, 512 + sl * 256, 256), [128, 8, 256], "pool")
            for mc in range(2):
                ps, pt = C.psum()
                C.mm(ps[:, 0:256], [(mT[:, c, mc * 128:(mc + 1) * 128], wb[:, c, :]) for c in range(8)], [wt] + mT_t, [pt])
                S.op("act", lambda e, ps=ps, mc=mc, sl=sl: e.copy(v[:, mc, sl * 256:(sl + 1) * 256], ps[:, 0:256]),
                     reads=[pt], writes=[kv_t])


class CrossAttn:
    def __init__(self, C):
        self.C = C
        self.pT = [C.alloc([128, 2, 512], ADT) for _ in range(2)]; self.pT_t = TL(2)
        self.rd = [C.alloc([128, 512]) for _ in range(2)]; self.rd_t = TL(2)
        self.k = 0

    def run(self, h, tg, q_ap, q_reads, kT, v, kv_t, out_ap, out_writes):
        C = self.C; S = C.S
        i = self.k % 2; self.k += 1
        pT = self.pT[i]; rd = self.rd[i]
        for mc in range(2):
            ps, pt = C.psum()
            C.mm(ps, [(kT[:, h, mc * 128:(mc + 1) * 128], q_ap)], [kv_t] + q_reads, [pt])
            S.op("act", lambda e, ps=ps, mc=mc: e.activation(pT[:, mc, :], ps, AF.Exp, scale=SCALE), reads=[pt], writes=[self.pT_t[i]])
        pd, pdt = C.psum()
        C.mm(pd, [(C.ones_b, pT[:, 0, :]), (C.ones_b, pT[:, 1, :])], [self.pT_t[i], C.ct], [pdt])
        po, pot = C.psum()
        C.mm(po, [(v[:, 0, h * 128:(h + 1) * 128], pT[:, 0, :]), (v[:, 1, h * 128:(h + 1) * 128], pT[:, 1, :])],
             [self.pT_t[i], kv_t], [pot])
        S.op("act", lambda e: e.activation(rd, pd, AF.Ln), reads=[pdt], writes=[self.rd_t[i]])
        S.op("act", lambda e: e.activation(rd, rd, AF.Exp, scale=-1.0), reads=[self.rd_t[i]], writes=[self.rd_t[i]])
        S.op("dve", lambda e: e.tensor_tensor(out_ap, po, rd, ALU.mult), reads=[pot, self.rd_t[i]], writes=out_writes)


def out_proj(C, mixT, mixT_t, wout_ap, x_in, x_out):
    S = C.S
    with C.scope():
        ws = WStream(C, nstage=3, nslot=4)
        yacc = [C.alloc([128, 4, 1024], F32) for _ in range(2)]; yacc_t = [TL(8), TL(8)]
        xin_v = x_in.rearrange("(t p) d -> p t d", p=128)
        xout_v = x_out.rearrange("(t p) d -> p t d", p=128)
        for tg in range(4):
            ya = yacc[tg % 2]; ya_t = yacc_t[tg % 2]
            S.dma("sp", ya, xin_v[:, tg * 4:(tg + 1) * 4, :], writes=ya_t)
            for s in range(4):
                wb, wt = ws.issue(wview(wout_ap, 2 * s, 2, 0, 1024), [128, 2, 1024], "act")

                def dn(e, s=s, wb=wb, tg=tg):
                    inst = None
                    for j in range(2):
                        k = 2 * s + j
                        for tt in range(4):
                            t0 = tg * 512 + tt * 128
                            for half in range(2):
                                inst = e.matmul(C.banks[tt * 2 + half], mixT[:, k, t0:t0 + 128],
                                                wb[:, j, half * 512:(half + 1) * 512], start=(k == 0), stop=(k == 7))
                    return inst
                S.op("pe", dn, reads=[wt, mixT_t[2 * s][tg], mixT_t[2 * s + 1][tg]], writes=C.bank_t)
            for tt in range(4):
                for half in range(2):
                    b = tt * 2 + half
                    S.op("dve", lambda e, b=b, tt=tt, half=half, ya=ya: e.tensor_tensor(
                        ya[:, tt, half * 512:(half + 1) * 512], ya[:, tt, half * 512:(half + 1) * 512], C.banks[b], ALU.add),
                        reads=[C.bank_t[b], ya_t[b]], writes=[ya_t[b]])
            S.dma("sp", xout_v[:, tg * 4:(tg + 1) * 4, :], ya, reads=ya_t)


def load_col(C, ap128):
    t = T()
    sb = C.alloc([128, 1])
    C.S.dma("sp", sb, ap128, writes=[t])
    return sb, t


def stage_mix1(C, x_in, x_out, mem, W):
    S = C.S
    Win = W["at_w_in"]
    with C.scope():
        gk_col, gk_t = load_col(C, W["xk_norm1"])
        gxq_col, gxq_t = load_col(C, W["xq_norm1"])
        gaq_col, gaq_t = load_col(C, W["at_q_norm"])
        gak_col, gak_t = load_col(C, W["at_k_norm"])
        kT = C.alloc([128, 4, MEM], ADT); v = C.alloc([128, 2, 512], ADT); kv_t = T()
        mem_kv(C, mem, W["norm_mem1"], W["w_mem_kv1"], gk_col, gk_t, kT, v, kv_t)
        mixT = C.alloc([128, 8, TOK], ADT); mixT_t = [TL(4) for _ in range(8)]
        with C.scope():
            hT = C.alloc([128, 8, TOK], ADT); hT_t = TL(NT)
            norm_T(C, x_in, TOK, W["norm_mix1"], hT, hT_t)
            ws = WStream(C, nstage=3, nslot=4, elems=1024)
            hn = HeadNorm(C)
            ca = CrossAttn(C)
            qh = C.alloc([128, TOK], ADT); qh_t = TL(4)
            for h in range(4):
                wb, wt = ws.issue(wview(Win, 0, 8, 4608 + h * 128, 128), [128, 8, 128], "pool")
                for tg in range(4):
                    tok = slice(tg * 512, (tg + 1) * 512)
                    ps, pt = C.psum()
                    C.mm(ps, [(wb[:, c, :], hT[:, c, tok]) for c in range(8)], [wt] + hT_t[tg * 4:tg * 4 + 4], [pt])
                    hn.run(ps, pt, 512, gxq_col, gxq_t, qh[:, tok], [qh_t[tg]])
                    ca.run(h, tg, qh[:, tok], [qh_t[tg]], kT, v, kv_t, mixT[:, 4 + h, tok], [mixT_t[4 + h][tg]])
            BT = C.alloc([128, 12, 256], F32); BT_t = TL(12)
            tab = C.alloc([128, 32 * 12], F32); tab_t = T()
            S.dma("sp", tab, W["rel_bias"].rearrange("b h -> (b h)").partition_broadcast(128), writes=[tab_t])
            negm = C.alloc([128, 256], F32); negm_t = T()
            S.dma("sp", negm, W["c_negmask"], writes=[negm_t])
            for gh in range(12):
                S.op("pool", lambda e, gh=gh: e.tensor_copy(BT[:, gh, :], negm), reads=[negm_t], writes=[BT_t[gh]])
            oh = [C.alloc([128, 256], F32) for _ in range(2)]; oh_t = TL(2)
            kk = 0
            for g in range(3):
                for b in W["_buckets"][g]:
                    o = oh[kk % 2]; ot = oh_t[kk % 2]; kk += 1
                    S.dma("sp", o, W["c_oh"][g, b], writes=[ot])
                    for h in range(4):
                        gh = g * 4 + h
                        S.op("dve", lambda e, o=o, gh=gh, b=b: e.scalar_tensor_tensor(
                            BT[:, gh, :], o, tab[:, b * 12 + gh:b * 12 + gh + 1], BT[:, gh, :], ALU.mult, ALU.add),
                            reads=[ot, tab_t, BT_t[gh]], writes=[BT_t[gh]])
            acc_o = C.alloc([128, TOK], F32); acc_d = C.alloc([128, TOK], F32); acc_t = T()
            qg = C.alloc([128, TOK], ADT); qg_t = TL(4)
            kg = C.alloc([128, TOK], ADT); kg_t = TL(4)
            vg = C.alloc([128, 16, 128], ADT); vg_t = TL(16)
            tS = [C.alloc([128, 256], F32) for _ in range(2)]; tS_t = TL(2)
            pT = [C.alloc([128, 256], ADT) for _ in range(2)]; pT_t = TL(2)
            kc = 0
            for h in range(4):
                S.op("pool", lambda e: e.memset(acc_o, 0.0), writes=[acc_t])
                S.op("pool", lambda e: e.memset(acc_d, 0.0), writes=[acc_t])
                for g, (win, r) in enumerate(GROUPS):
                    L = TOK // r
                    nj = L // 128
                    cq = ((0 * 3 + g) * 4 + h) * 128
                    ck = ((1 * 3 + g) * 4 + h) * 128
                    cv = ((2 * 3 + g) * 4 + h) * 128
                    wq, wqt = ws.issue(wview(Win, 0, 8, cq, 128), [128, 8, 128], "pool")
                    for tg in range(4):
                        tok = slice(tg * 512, (tg + 1) * 512)
                        ps, pt = C.psum()
                        C.mm(ps, [(wq[:, c, :], hT[:, c, tok]) for c in range(8)], [wqt] + hT_t[tg * 4:tg * 4 + 4], [pt])
                        hn.run(ps, pt, 512, gaq_col, gaq_t, qg[:, tok], [qg_t[tg]])
                    wk, wkt = ws.issue(wview(Win, 0, 8, ck, 128), [128, 8, 128], "pool")
                    for tg in range(4):
                        tok = slice(tg * 512, (tg + 1) * 512)
                        ps, pt = C.psum()
                        C.mm(ps, [(wk[:, c, :], hT[:, c, tok]) for c in range(8)], [wkt] + hT_t[tg * 4:tg * 4 + 4], [pt])
                        hn.run(ps, pt, 512, gak_col, gak_t, kg[:, tok], [kg_t[tg]])
                    wv, wvt = ws.issue(wview(Win, 0, 8, cv, 128), [128, 8, 128], "pool")
                    for r0 in range(r):
                        for j in range(nj):
                            idx = r0 * nj + j
                            a0 = r0 + r * 128 * j
                            a1 = a0 + r * 127 + 1
                            ps, pt = C.psum()
                            C.mm(ps[:, 0:128], [(hT[:, c, a0:a1:r], wv[:, c, :]) for c in range(8)], [wvt] + hT_t, [pt])
                            S.op("act", lambda e, ps=ps, idx=idx: e.copy(vg[:, idx, :], ps[:, 0:128]), reads=[pt], writes=[vg_t[idx]])
                    for r0 in range(r):
                        for j in range(nj):
                            idx = r0 * nj + j
                            a0 = r0 + r * 128 * j
                            a1 = a0 + r * 127 + 1
                            q0 = max(0, 128 * j - 64); q1 = min(L, 128 * j + 192)
                            N = q1 - q0
                            n0 = q0 - (128 * j - 64)
                            b0 = r0 + r * q0
                            b1 = b0 + r * (N - 1) + 1
                            i = kc % 2; kc += 1
                            ps, pt = C.psum()
                            C.mm(ps[:, 0:N], [(kg[:, a0:a1:r], qg[:, b0:b1:r])], kg_t + qg_t, [pt])
                            S.op("dve", lambda e, ps=ps, i=i, N=N, n0=n0, g=g, h=h: e.scalar_tensor_tensor(
                                tS[i][:, 0:N], ps[:, 0:N], SCALE, BT[:, g * 4 + h, n0:n0 + N], ALU.mult, ALU.add),
                                reads=[pt, BT_t[g * 4 + h]], writes=[tS_t[i]])
                            S.op("act", lambda e, i=i, N=N: e.activation(pT[i][:, 0:N], tS[i][:, 0:N], AF.Exp),
                                 reads=[tS_t[i]], writes=[pT_t[i]])
                            pd, pdt = C.psum()
                            C.mm(pd[:, 0:N], [(C.ones_b, pT[i][:, 0:N])], [pT_t[i], C.ct], [pdt])
                            po, pot = C.psum()
                            C.mm(po[:, 0:N], [(vg[:, idx, :], pT[i][:, 0:N])], [pT_t[i], vg_t[idx]], [pot])
                            S.op("dve", lambda e, pd=pd, N=N, b0=b0, b1=b1, r=r: e.tensor_tensor(
                                acc_d[:, b0:b1:r], acc_d[:, b0:b1:r], pd[:, 0:N], ALU.add), reads=[pdt, acc_t], writes=[acc_t])
                            S.op("dve", lambda e, po=po, N=N, b0=b0, b1=b1, r=r: e.tensor_tensor(
                                acc_o[:, b0:b1:r], acc_o[:, b0:b1:r], po[:, 0:N], ALU.add), reads=[pot, acc_t], writes=[acc_t])
                S.op("act", lambda e: e.activation(acc_d, acc_d, AF.Ln), reads=[acc_t], writes=[acc_t])
                S.op("act", lambda e: e.activation(acc_d, acc_d, AF.Exp, scale=-1.0), reads=[acc_t], writes=[acc_t])
                for tg in range(4):
                    tok = slice(tg * 512, (tg + 1) * 512)
                    S.op("dve", lambda e, tok=tok, h=h: e.tensor_tensor(mixT[:, h, tok], acc_o[:, tok], acc_d[:, tok], ALU.mult),
                         reads=[acc_t], writes=[mixT_t[h][tg]])
        out_proj(C, mixT, mixT_t, W["w_out1"], x_in, x_out)


NFFT = 4096
WSC = 2.0 / NFFT


def hyena_filter(C, W, o, h2T, h2_t, pnyq, pnyq_t, altcol, alt_t):
    S = C.S
    with C.scope():
        w3o = C.alloc([64, 1024], F32); w3_t = T()
        S.dma("sp", w3o, W["hy_filt_w3"][:, o * 1024:(o + 1) * 1024], writes=[w3_t])
        Gp = C.alloc([128, 16, 512], ADT); Gm = C.alloc([128, 16, 512], ADT); G_t = TL(16)
        dk = [C.alloc([128, 512], F32) for _ in range(2)]; dk_t = TL(2)
        kf = [C.alloc([128, 512], F32) for _ in range(2)]; kb = [C.alloc([128, 512], F32) for _ in range(2)]; kk_t = TL(2)
        ab = [C.alloc([128, 2, 512], ADT) for _ in range(2)]; ab_t = TL(2)
        nb = C.reserve()
        nps, npt = C.banks[nb], C.bank_t[nb]
        for dc in range(16):
            i = dc % 2
            S.dma("sp", dk[i], W["c_decay"][dc * 128:(dc + 1) * 128, :], writes=[dk_t[i]])
            pf, pft = C.psum()
            C.mm(pf, [(h2T[:, dc * 128:(dc + 1) * 128], w3o[:, 0:512])], [h2_t, w3_t], [pft])
            pb, pbt = C.psum()
            C.mm(pb, [(h2T[:, dc * 128:(dc + 1) * 128], w3o[:, 512:1024])], [h2_t, w3_t], [pbt])
            S.op("dve", lambda e, i=i, pf=pf: e.tensor_tensor(kf[i], pf, dk[i], ALU.mult), reads=[pft, dk_t[i]], writes=[kk_t[i]])
            S.op("dve", lambda e, i=i, pb=pb: e.tensor_tensor(kb[i], pb, dk[i], ALU.mult), reads=[pbt, dk_t[i], kk_t[i]], writes=[kk_t[i]])
            if dc == 0:
                S.op("dve", lambda e, i=i: e.memset(kb[i][0:1, :], 0.0), reads=[kk_t[i]], writes=[kk_t[i]])
            S.op("pool", lambda e, i=i, dc=dc: e.tensor_tensor(Gp[:, dc, :], kf[i], kb[i], ALU.add), reads=[kk_t[i]], writes=[G_t[dc]])
            S.op("pool", lambda e, i=i, dc=dc: e.tensor_tensor(Gm[:, dc, :], kf[i], kb[i], ALU.subtract), reads=[kk_t[i], G_t[dc]], writes=[G_t[dc]])
            S.op("act", lambda e, i=i: e.activation(ab[i][:, 0, :], kf[i], AF.Abs), reads=[kk_t[i]], writes=[ab_t[i]])
            S.op("act", lambda e, i=i: e.activation(ab[i][:, 1, :], kb[i], AF.Abs), reads=[kk_t[i], ab_t[i]], writes=[ab_t[i]])

            def nm(e, i=i, dc=dc):
                e.matmul(nps, C.ones_b, ab[i][:, 0, :], start=(dc == 0), stop=False)
                return e.matmul(nps, C.ones_b, ab[i][:, 1, :], start=False, stop=(dc == 15))
            S.op("pe", nm, reads=[ab_t[i], C.ct], writes=[npt])
        rn = C.alloc([128, 512], F32); sk = C.alloc([128, 512], F32); rn_t = T()
        S.dma("sp", sk, W["hy_skip"][o].partition_broadcast(128), writes=[rn_t])
        S.op("dve", lambda e: e.tensor_scalar(rn, nps, 1e-6, None, ALU.add), reads=[npt], writes=[rn_t])
        S.op("dve", lambda e: e.reciprocal(rn, rn), reads=[rn_t], writes=[rn_t])
        S.op("dve", lambda e: e.tensor_scalar(rn, rn, WSC, None, ALU.mult), reads=[rn_t], writes=[rn_t])
        S.op("dve", lambda e: e.tensor_scalar(sk, sk, WSC, None, ALU.mult), reads=[rn_t], writes=[rn_t])
        C.release(nb)
        if HY_F <= 1:
            return
        pn, pnt = C.psum()
        C.mm(pn, [(altcol, Gp[:, dc, :]) for dc in range(16)], G_t + [alt_t], [pnt])
        S.op("dve", lambda e: e.tensor_tensor(pnyq[0:1, o, :], pn[0:1, :], rn[0:1, :], ALU.mult), reads=[pnt, rn_t], writes=[pnyq_t])
        S.op("dve", lambda e: e.tensor_tensor(pnyq[0:1, o, :], pnyq[0:1, o, :], sk[0:1, :], ALU.add), reads=[pnyq_t, rn_t], writes=[pnyq_t])
        S.op("dve", lambda e: e.tensor_scalar(pnyq[0:1, o, :], pnyq[0:1, o, :], 0.5, None, ALU.mult), reads=[pnyq_t], writes=[pnyq_t])
        if HY_F <= 2:
            return
        tb = [C.alloc([128, 16, 256], ADT) for _ in range(4)]; tb_t = TL(4)
        tmp = [C.alloc([128, 512], F32) for _ in range(2)]; tmp_t = TL(2)
        st = [C.alloc([128, 512], ADT) for _ in range(4)]; st_t = TL(4)
        k = 0
        for fb in range(8):
            cb, cbt = tb[(2 * fb) % 4], tb_t[(2 * fb) % 4]
            sb, sbt = tb[(2 * fb + 1) % 4], tb_t[(2 * fb + 1) % 4]
            S.dma("sp", cb, W["c_dftc"][fb], writes=[cbt])
            S.dma("sp", sb, W["c_dfts"][fb], writes=[sbt])
            for j in range(2):
                fc = 2 * fb + j
                pp, ppt = C.psum()
                C.mm(pp, [(cb[:, dc, j * 128:(j + 1) * 128], Gp[:, dc, :]) for dc in range(16)], G_t + [cbt], [ppt])
                pq, pqt = C.psum()
                C.mm(pq, [(sb[:, dc, j * 128:(j + 1) * 128], Gm[:, dc, :]) for dc in range(16)], G_t + [sbt], [pqt])
                i = k % 2; a = (2 * k) % 4; b = (2 * k + 1) % 4; k += 1
                S.op("dve", lambda e, i=i, pp=pp: e.tensor_tensor(tmp[i], pp, rn, ALU.mult), reads=[ppt, rn_t], writes=[tmp_t[i]])
                S.op("dve", lambda e, i=i, a=a: e.tensor_tensor(st[a], tmp[i], sk, ALU.add), reads=[tmp_t[i], rn_t], writes=[st_t[a]])
                if fc == 0:
                    S.op("dve", lambda e, a=a: e.tensor_scalar(st[a][0:1, :], st[a][0:1, :], 0.5, None, ALU.mult), reads=[st_t[a]], writes=[st_t[a]])
                S.op("dve", lambda e, b=b, pq=pq: e.tensor_tensor(st[b], pq, rn, ALU.mult), reads=[pqt, rn_t], writes=[st_t[b]])
                S.dma("sp", W["hy_pq"][o, 0, fc], st[a], reads=[st_t[a]], writes=[W["hy_pq_t"]])
                S.dma("sp", W["hy_pq"][o, 1, fc], st[b], reads=[st_t[b]], writes=[W["hy_pq_t"]])


def hyena_conv(C, W, o, u, u_t, pnyq, pnyq_t, altcol, altrow, alt_t, out_tok=None, out_tok_t=None, mixT=None, mixT_t=None):
    S = C.S
    with C.scope():
        tb = [C.alloc([128, 16, 256], ADT) for _ in range(4)]; tb_t = TL(4)
        pqs = [C.alloc([128, 2, 2, 512], ADT) for _ in range(2)]; pqs_t = TL(2)
        Rr = C.alloc([128, 16, 512], ADT); Ri = C.alloc([128, 16, 512], ADT); R_t = TL(16)
        t1 = [C.alloc([128, 512], F32) for _ in range(2)]; t2 = [C.alloc([128, 512], F32) for _ in range(2)]; tt_t = TL(2)
        rny = C.alloc([128, 512], ADT); rny_t = T()
        S.op("pool", lambda e: e.memset(rny, 0.0), writes=[rny_t])
        gt = [C.alloc([128, 512], ADT) for _ in range(2)]; gt_t = TL(2)
        k = 0
        for fb in range(8):
            cb, cbt = tb[(2 * fb) % 4], tb_t[(2 * fb) % 4]
            sb, sbt = tb[(2 * fb + 1) % 4], tb_t[(2 * fb + 1) % 4]
            S.dma("sp", cb, W["c_dftc"][fb], writes=[cbt])
            S.dma("sp", sb, W["c_dfts"][fb], writes=[sbt])
            pq = pqs[fb % 2]; pq_t = pqs_t[fb % 2]
            S.dma("sp", pq[:, 0], W["hy_pq"][o, 0, 2 * fb:2 * fb + 2].rearrange("f p c -> p f c"), reads=[W["hy_pq_t"]], writes=[pq_t])
            S.dma("sp", pq[:, 1], W["hy_pq"][o, 1, 2 * fb:2 * fb + 2].rearrange("f p c -> p f c"), reads=[W["hy_pq_t"]], writes=[pq_t])
            for j in range(2):
                fc = 2 * fb + j
                pa, pat = C.psum()
                C.mm(pa, [(cb[:, sc, j * 128:(j + 1) * 128], u[:, sc, :]) for sc in range(16)], u_t + [cbt], [pat])
                pb, pbt = C.psum()
                C.mm(pb, [(sb[:, sc, j * 128:(j + 1) * 128], u[:, sc, :]) for sc in range(16)], u_t + [sbt], [pbt])
                i = k % 2; k += 1
                P = pq[:, 0, j, :]; Q = pq[:, 1, j, :]
                S.op("dve", lambda e, i=i, pa=pa, P=P: e.tensor_tensor(t1[i], pa, P, ALU.mult), reads=[pat, pq_t], writes=[tt_t[i]])
                S.op("dve", lambda e, i=i, pb=pb, Q=Q: e.tensor_tensor(t2[i], pb, Q, ALU.mult), reads=[pbt, pq_t, tt_t[i]], writes=[tt_t[i]])
                S.op("pool", lambda e, i=i, fc=fc: e.tensor_tensor(Rr[:, fc, :], t1[i], t2[i], ALU.subtract), reads=[tt_t[i]], writes=[R_t[fc]])
                S.op("dve", lambda e, i=i, pa=pa, Q=Q: e.tensor_tensor(t1[i], pa, Q, ALU.mult), reads=[pat, pq_t, tt_t[i]], writes=[tt_t[i]])
                S.op("dve", lambda e, i=i, pb=pb, P=P: e.tensor_tensor(t2[i], pb, P, ALU.mult), reads=[pbt, pq_t, tt_t[i]], writes=[tt_t[i]])
                S.op("pool", lambda e, i=i, fc=fc: e.tensor_tensor(Ri[:, fc, :], t1[i], t2[i], ALU.add), reads=[tt_t[i], R_t[fc]], writes=[R_t[fc]])
        pn, pnt = C.psum()
        C.mm(pn, [(altcol, u[:, sc, :]) for sc in range(16)], u_t + [alt_t], [pnt])
        S.op("dve", lambda e: e.tensor_tensor(rny[0:1, :], pn[0:1, :], pnyq[0:1, o, :], ALU.mult), reads=[pnt, pnyq_t, rny_t], writes=[rny_t])
        for tbk in range(8):
            cb, cbt = tb[(2 * tbk) % 4], tb_t[(2 * tbk) % 4]
            sb, sbt = tb[(2 * tbk + 1) % 4], tb_t[(2 * tbk + 1) % 4]
            S.dma("sp", cb, W["c_dftc"][tbk], writes=[cbt])
            S.dma("sp", sb, W["c_dfts"][tbk], writes=[sbt])
            if o == 0:
                for j in range(2):
                    tc = 2 * tbk + j
                    g = gt[tc % 2]; g_t = gt_t[tc % 2]
                    S.dma("sp", g, W["hy_c1"][tc], reads=[W["hy_c1_t"]], writes=[g_t])
                    ps, pt = C.psum()
                    pairs = [(cb[:, fc, j * 128:(j + 1) * 128], Rr[:, fc, :]) for fc in range(16)]
                    pairs += [(sb[:, fc, j * 128:(j + 1) * 128], Ri[:, fc, :]) for fc in range(16)]
                    pairs += [(altrow[:, 0:128], rny[:, :])]
                    C.mm(ps, pairs, R_t + [cbt, sbt, rny_t, alt_t], [pt])
                    S.op("dve", lambda e, ps=ps, g=g, tc=tc: e.tensor_tensor(out_tok[:, tc, :], ps, g, ALU.mult),
                         reads=[pt, g_t], writes=[out_tok_t[tc]])
            else:
                for cc in range(4):
                    g = gt[cc % 2]; g_t = gt_t[cc % 2]
                    S.dma("sp", g[:, 0:256], W["hy_c2T"][cc][:, tbk * 256:(tbk + 1) * 256], reads=[W["hy_c2T_t"]], writes=[g_t])
                    ps, pt = C.psum()
                    pairs = [(Rr[:, fc, cc * 128:(cc + 1) * 128], cb[:, fc, :]) for fc in range(16)]
                    pairs += [(Ri[:, fc, cc * 128:(cc + 1) * 128], sb[:, fc, :]) for fc in range(16)]
                    pairs += [(rny[:, cc * 128:(cc + 1) * 128], altrow[:, 0:256])]
                    C.mm(ps[:, 0:256], pairs, R_t + [cbt, sbt, rny_t, alt_t], [pt])
                    S.op("dve", lambda e, ps=ps, g=g, cc=cc, tbk=tbk: e.tensor_tensor(
                        mixT[:, cc, tbk * 256:(tbk + 1) * 256], ps[:, 0:256], g[:, 0:256], ALU.mult),
                        reads=[pt, g_t], writes=[mixT_t[cc][tbk // 2]])


def stage_mix0(C, x_in, x_out, mem, W):
    S = C.S
    Win = W["hy_w_in"]
    for nm in ("hy_pq", "hy_c1", "hy_c2T", "hy_u0"):
        W[nm + "_t"] = T()
    with C.scope():
        gk_col, gk_t = load_col(C, W["xk_norm0"])
        gxq_col, gxq_t = load_col(C, W["xq_norm0"])
        kT = C.alloc([128, 4, MEM], ADT); v = C.alloc([128, 2, 512], ADT); kv_t = T()
        mem_kv(C, mem, W["norm_mem0"], W["w_mem_kv0"], gk_col, gk_t, kT, v, kv_t)
        mixT = C.alloc([128, 8, TOK], ADT); mixT_t = [TL(4) for _ in range(8)]
        altcol = C.alloc([128, 128], ADT); altrow = C.alloc([128, 512], ADT); alt_t = T()
        altf = C.alloc([128, 128], F32); altrf = C.alloc([128, 512], F32)
        S.dma("sp", altf, W["c_altcol"], writes=[alt_t])
        S.dma("sp", altrf, W["c_altrow"], writes=[alt_t])
        S.op("dve", lambda e: e.tensor_copy(altcol, altf), reads=[alt_t], writes=[alt_t])
        S.op("dve", lambda e: e.tensor_copy(altrow, altrf), reads=[alt_t], writes=[alt_t])
        pnyq = C.alloc([1, 2, 512], F32); pnyq_t = T()
        with C.scope():
            hT = C.alloc([128, 8, TOK], ADT); hT_t = TL(NT)
            norm_T(C, x_in, TOK, W["norm_mix0"], hT, hT_t)
            ws = WStream(C, nstage=3, nslot=4, elems=1024)
            hn = HeadNorm(C)
            ca = CrossAttn(C)
            qh = C.alloc([128, TOK], ADT); qh_t = TL(4)
            for h in range(4):
                wb, wt = ws.issue(wview(Win, 0, 8, 1536 + h * 128, 128), [128, 8, 128], "pool")
                for tg in range(4):
                    tok = slice(tg * 512, (tg + 1) * 512)
                    ps, pt = C.psum()
                    C.mm(ps, [(wb[:, c, :], hT[:, c, tok]) for c in range(8)], [wt] + hT_t[tg * 4:tg * 4 + 4], [pt])
                    hn.run(ps, pt, 512, gxq_col, gxq_t, qh[:, tok], [qh_t[tg]])
                    ca.run(h, tg, qh[:, tok], [qh_t[tg]], kT, v, kv_t, mixT[:, 4 + h, tok], [mixT_t[4 + h][tg]])
            cwb = C.alloc([128, 12, 4], F32); cwb_t = T()
            S.dma("sp", cwb, W["hy_cwb"], writes=[cwb_t])
            zp = [C.alloc([128, TOK + 2], F32) for _ in range(2)]; zp_t = TL(2)
            for i in range(2):
                S.op("pool", lambda e, i=i: e.memset(zp[i], 0.0), writes=[zp_t[i]])
            ctmp = C.alloc([128, TOK], F32); ctmp_t = T()
            cv = [C.alloc([128, TOK], ADT) for _ in range(2)]; cv_t = TL(2)
            stg = [C.alloc([128, 4, 128], ADT) for _ in range(2)]; stg_t = TL(2)
            kst = 0
            for ch in range(12):
                i = ch % 2
                wb, wt = ws.issue(wview(Win, 0, 8, ch * 128, 128), [128, 8, 128], "pool")
                for tg in range(4):
                    tok = slice(tg * 512, (tg + 1) * 512)
                    ps, pt = C.psum()
                    C.mm(ps, [(wb[:, c, :], hT[:, c, tok]) for c in range(8)], [wt] + hT_t[tg * 4:tg * 4 + 4], [pt])
                    S.op("act", lambda e, ps=ps, i=i, tg=tg: e.copy(zp[i][:, 1 + tg * 512:1 + (tg + 1) * 512], ps), reads=[pt], writes=[zp_t[i]])
                z = zp[i]
                S.op("dve", lambda e, z=z, ch=ch: e.tensor_scalar(ctmp, z[:, 1:TOK + 1], cwb[:, ch, 1:2], cwb[:, ch, 3:4], ALU.mult, ALU.add),
                     reads=[zp_t[i], cwb_t], writes=[ctmp_t])
                S.op("dve", lambda e, z=z, ch=ch: e.scalar_tensor_tensor(ctmp, z[:, 0:TOK], cwb[:, ch, 0:1], ctmp, ALU.mult, ALU.add),
                     reads=[zp_t[i], cwb_t, ctmp_t], writes=[ctmp_t])
                S.op("dve", lambda e, z=z, ch=ch, i=i: e.scalar_tensor_tensor(cv[i], z[:, 2:TOK + 2], cwb[:, ch, 2:3], ctmp, ALU.mult, ALU.add),
                     reads=[zp_t[i], cwb_t, ctmp_t], writes=[cv_t[i]])
                cc = ch % 4
                if ch >= 8:
                    S.dma("sp", W["hy_c2T"][cc], cv[i], reads=[cv_t[i]], writes=[W["hy_c2T_t"]])
                else:
                    dst = W["hy_u0"] if ch < 4 else W["hy_c1"]
                    dst_t = W["hy_u0_t"] if ch < 4 else W["hy_c1_t"]
                    for q4 in range(4):
                        ps, pt = C.psum()
                        psb = ps.bitcast(ADT)

                        def tr(e, psb=psb, i=i, q4=q4):
                            inst = None
                            for jj in range(4):
                                sc = q4 * 4 + jj
                                inst = e.transpose(psb[:, jj * 128:(jj + 1) * 128], cv[i][:, sc * 128:(sc + 1) * 128], C.ident_b)
                            return inst
                        S.op("pe", tr, reads=[cv_t[i], C.ct], writes=[pt])
                        si = kst % 2; kst += 1
                        S.op("act", lambda e, psb=psb, si=si: e.copy(stg[si], psb[:, 0:512].rearrange("p (a b) -> p a b", a=4)),
                             reads=[pt], writes=[stg_t[si]])
                        S.dma("sp", dst[q4 * 4:q4 * 4 + 4, :, cc * 128:(cc + 1) * 128].rearrange("s p c -> p s c"), stg[si],
                              reads=[stg_t[si]], writes=[dst_t])
        h2T = C.alloc([64, TOK], F32); h2_t = T()
        if HY_STOP <= 1:
            out_proj(C, mixT, mixT_t, W["w_out0"], x_in, x_out)
            return
        with C.scope():
            featsT = C.alloc([33, TOK], F32); f_t = T()
            S.dma("sp", featsT, W["c_featsT"], writes=[f_t])
            w1 = C.alloc([33, 64], F32); w2 = C.alloc([64, 64], F32); sm = C.alloc([64, 8], F32); sm_t = T()
            S.dma("sp", w1, W["hy_filt_w1"], writes=[sm_t])
            S.dma("sp", w2, W["hy_filt_w2"], writes=[sm_t])
            S.dma("sp", sm[:, 0:3], W["hy_fsm"], writes=[sm_t])
            S.op("dve", lambda e: e.tensor_tensor(sm[:, 3:4], sm[:, 0:1], sm[:, 1:2], ALU.mult), reads=[sm_t], writes=[sm_t])
            S.op("dve", lambda e: e.tensor_tensor(sm[:, 4:5], sm[:, 0:1], sm[:, 2:3], ALU.mult), reads=[sm_t], writes=[sm_t])
            h1T = C.alloc([64, TOK], F32); h1_t = T()
            pre = C.alloc([64, 512], F32); m1 = C.alloc([64, 512], F32); pre_t = T()
            PI = math.pi
            for layer in range(2):
                src, src_t, wl, dst, dst_t, fbcol = ((featsT, f_t, w1, h1T, h1_t, 3) if layer == 0 else (h1T, h1_t, w2, h2T, h2_t, 4))
                for tg in range(4):
                    tok = slice(tg * 512, (tg + 1) * 512)
                    ps, pt = C.psum()
                    C.mm(ps[0:64, :], [(wl, src[:, tok])], [sm_t, src_t], [pt])
                    S.op("dve", lambda e, ps=ps, fbcol=fbcol: e.tensor_scalar(pre, ps[0:64, :], sm[:, 0:1], sm[:, fbcol:fbcol + 1], ALU.mult, ALU.add),
                         reads=[pt, sm_t], writes=[pre_t])
                    for rep in range(2):
                        S.op("dve", lambda e: e.tensor_scalar(m1, pre, PI, -2 * PI, ALU.is_gt, ALU.mult), reads=[pre_t], writes=[pre_t])
                        S.op("dve", lambda e: e.tensor_tensor(pre, pre, m1, ALU.add), reads=[pre_t], writes=[pre_t])
                        S.op("dve", lambda e: e.tensor_scalar(m1, pre, -PI, 2 * PI, ALU.is_lt, ALU.mult), reads=[pre_t], writes=[pre_t])
                        S.op("dve", lambda e: e.tensor_tensor(pre, pre, m1, ALU.add), reads=[pre_t], writes=[pre_t])
                    S.op("act", lambda e, dst=dst, tok=tok: e.activation(dst[:, tok], pre, AF.Sin), reads=[pre_t], writes=[dst_t])
        if HY_STOP <= 2:
            out_proj(C, mixT, mixT_t, W["w_out0"], x_in, x_out)
            return
        hyena_filter(C, W, 0, h2T, h2_t, pnyq, pnyq_t, altcol, alt_t)
        if HY_STOP <= 3:
            out_proj(C, mixT, mixT_t, W["w_out0"], x_in, x_out)
            return
        u1 = C.alloc([128, 16, 512], ADT); u1_t = TL(16)
        with C.scope():
            u0 = C.alloc([128, 16, 512], ADT); u0_t = TL(16)
            S.dma("sp", u0, W["hy_u0"].rearrange("s p c -> p s c"), reads=[W["hy_u0_t"]], writes=u0_t)
            hyena_conv(C, W, 0, u0, u0_t, pnyq, pnyq_t, altcol, altrow, alt_t, out_tok=u1, out_tok_t=u1_t)
        if HY_STOP <= 4:
            out_proj(C, mixT, mixT_t, W["w_out0"], x_in, x_out)
            return
        hyena_filter(C, W, 1, h2T, h2_t, pnyq, pnyq_t, altcol, alt_t)
        hyena_conv(C, W, 1, u1, u1_t, pnyq, pnyq_t, altcol, altrow, alt_t, mixT=mixT, mixT_t=mixT_t)
        out_proj(C, mixT, mixT_t, W["w_out0"], x_in, x_out)


def host_consts():
    c = {}
    c["c_ident"] = np.eye(128, dtype=np.float32)
    sel = np.zeros((8, NE * 128), np.float32)
    for e in range(NE):
        sel[e, e * 128:(e + 1) * 128] = 1.0
    c["c_sel"] = sel
    c["c_iota"] = np.arange(CAP, dtype=np.float32)
    c["c_slotcol"] = (np.arange(128, dtype=np.float32)[:, None] + 128.0 * np.arange(NSL, dtype=np.float32)[None, :]).astype(np.float32)
    c["c_tri"] = np.triu(np.ones((128, 128), np.float32), k=1)
    i = np.arange(128)[:, None]; n = np.arange(256)[None, :]
    rel = i - n + 64
    band = np.abs(rel) <= 64
    c["c_negmask"] = np.where(band, 0.0, NEG).astype(np.float32)
    oh = np.zeros((3, 32, 128, 256), np.float32)
    buckets = []
    for g, (win, r) in enumerate(GROUPS):
        bk = _rel_bucket(rel * r)
        used = []
        for b in range(32):
            m = band & (bk == b)
            if m.any():
                oh[g, b] = m
                used.append(b)
        buckets.append(used)
    c["c_oh"] = oh
    c["_buckets"] = buckets
    import ml_dtypes
    L = TOK
    t = np.linspace(0.0, 1.0, L, dtype=np.float32)[:, None]
    f = np.linspace(1e-4, 15.0, 16, dtype=np.float32)[None]
    ang = (np.float32(2.0 * math.pi / L) * np.arange(L, dtype=np.float32)[:, None] * f).astype(np.float32)
    feats = np.concatenate([t, np.cos(ang), -np.sin(ang)], axis=-1).astype(np.float32)
    c["c_featsT"] = np.ascontiguousarray(feats.T)
    deltas = np.abs(np.linspace(math.log(1e-2) / 1.5, math.log(1e-2) / 0.3, 512, dtype=np.float32))
    c["c_decay"] = (np.exp(-t * deltas[None, :]) + 0.05).astype(np.float32)
    idx = np.outer(np.arange(L, dtype=np.int64), np.arange(L, dtype=np.int64)) % NFFT
    th = 2.0 * math.pi / NFFT
    for nm, fn in (("c_dftc", np.cos), ("c_dfts", np.sin)):
        full = fn(th * idx).astype(np.float32)
        til = full.reshape(16, 128, 8, 256).transpose(2, 1, 0, 3)
        c[nm] = np.ascontiguousarray(til).astype(ml_dtypes.bfloat16)
    sgn = np.where(np.arange(512) % 2 == 0, 1.0, -1.0).astype(np.float32)
    c["c_altcol"] = np.ascontiguousarray(np.repeat(sgn[:128, None], 128, axis=1))
    ar = np.zeros((128, 512), np.float32); ar[0] = sgn
    c["c_altrow"] = ar
    return c


def _rel_bucket(rel):
    half = 16
    exact = 8
    n = np.abs(rel)
    large = exact + (np.log(np.maximum(n, 1) / exact) / np.log(1024 / exact) * (half - exact)).astype(np.int32)
    large = np.minimum(large, half - 1)
    return (np.where(rel > 0, half, 0) + np.where(n < exact, n, large)).astype(np.int32)


def tile_gu(w):
    w = np.asarray(w, np.float32)
    E = w.shape[0]
    return np.ascontiguousarray(w.reshape(E, 8, 128, 11, 256).transpose(0, 3, 2, 1, 4))


def tile_d(w):
    w = np.asarray(w, np.float32)
    E = w.shape[0]
    wp = np.zeros((E, 24, 128, 2, 512), np.float32)
    wp[:, :22] = w.reshape(E, 22, 128, 2, 512)
    return np.ascontiguousarray(wp.reshape(E, 6, 4, 128, 2, 512).transpose(0, 4, 1, 3, 2, 5))


def pc(v):
    v = np.asarray(v, np.float32)
    return np.ascontiguousarray(v.reshape(-1, 128).T)


STAGE_INPUTS = {
    "ffn0": {"norm_ffn0": [128, 8], "ffn_w_gate": [1, 11, 128, 8, 256], "ffn_w_up": [1, 11, 128, 8, 256], "ffn_w_down": [1, 2, 6, 128, 4, 512]},
    "moe": {"norm_ffn1": [128, 8], "moe_router": [1024, NE], "moe_w_gate": [NE, 11, 128, 8, 256], "moe_w_up": [NE, 11, 128, 8, 256],
            "moe_w_down": [NE, 2, 6, 128, 4, 512], "c_sel": [8, NE * 128]},
    "moes": {"norm_ffn1": [128, 8], "norm_ffn1_row": [1024], "moe_router": [1024, NE], "moe_w_gate": [NE, 11, 128, 8, 256],
             "moe_w_up": [NE, 11, 128, 8, 256], "moe_w_down": [NE, 2, 6, 128, 4, 512], "c_iota": [CAP], "c_slotcol": [128, NSL], "c_tri": [128, 128]},
    "mix1": {"norm_mix1": [128, 8], "norm_mem1": [128, 8], "w_mem_kv1": [1024, 1024], "xq_norm1": [128, 1], "xk_norm1": [128, 1],
             "w_out1": [1024, 1024], "at_w_in": [1024, 5120], "at_q_norm": [128, 1], "at_k_norm": [128, 1], "rel_bias": [32, 12],
             "c_oh": [3, 32, 128, 256], "c_negmask": [128, 256], "mem": [MEM, D]},
    "mix0": {"norm_mix0": [128, 8], "norm_mem0": [128, 8], "w_mem_kv0": [1024, 1024], "xq_norm0": [128, 1], "xk_norm0": [128, 1],
             "w_out0": [1024, 1024], "hy_w_in": [1024, 2048], "hy_cwb": [128, 12, 4], "hy_fsm": [64, 3], "hy_filt_w1": [33, 64],
             "hy_filt_w2": [64, 64], "hy_filt_w3": [64, 2048], "hy_skip": [2, 512], "c_featsT": [33, TOK], "c_decay": [TOK, 512],
             "c_dftc": ([8, 128, 16, 256], "bf16"), "c_dfts": ([8, 128, 16, 256], "bf16"), "c_altcol": [128, 128], "c_altrow": [128, 512],
             "mem": [MEM, D]},
}
SCRATCH = {"hy_pq": [2, 2, 16, 128, 512], "hy_c1": [16, 128, 512], "hy_u0": [16, 128, 512], "hy_c2T": [4, 128, TOK]}


def build(stages):
    nc = bass.Bass("TRN2", target_bir_lowering=False)
    W = {}

    def ext(name, shape):
        dt = F32
        if isinstance(shape, tuple):
            shape, dt = shape[0], BF16
        if name not in W:
            W[name] = nc.dram_tensor(name, list(shape), dt, kind="ExternalInput").ap()
    if "mix0" in stages:
        for k, shp in SCRATCH.items():
            W[k] = nc.dram_tensor(k, list(shp), BF16, kind="Internal").ap()
    ext("c_ident", [128, 128])
    for st in stages:
        for k, shp in STAGE_INPUTS[st].items():
            ext(k, shp)
    xs = []
    for i in range(len(stages) + 1):
        if i == 0:
            xs.append(nc.dram_tensor("x_in", [TOK, D], F32, kind="ExternalInput").ap())
        elif i == len(stages):
            xs.append(nc.dram_tensor("x_out", [TOK, D], F32, kind="ExternalOutput").ap())
        else:
            xs.append(nc.dram_tensor("x_mid%d" % i, [TOK, D], F32, kind="Internal").ap())
    W["_buckets"] = host_consts()["_buckets"]
    with contextlib.ExitStack() as stack:
        C = Ctx(nc, stack)
        load_consts(C, W)
        for i, st in enumerate(stages):
            if st == "ffn0":
                stage_ffn(C, xs[i], xs[i + 1], W["norm_ffn0"], W["ffn_w_gate"], W["ffn_w_up"], W["ffn_w_down"])
            elif st == "moe":
                stage_ffn(C, xs[i], xs[i + 1], W["norm_ffn1"], W["moe_w_gate"], W["moe_w_up"], W["moe_w_down"],
                          router=W["moe_router"], sel=W["c_sel"])
            elif st == "moes":
                stage_moe_sparse(C, xs[i], xs[i + 1], W["norm_ffn1"], W["norm_ffn1_row"], W["moe_w_gate"], W["moe_w_up"],
                                 W["moe_w_down"], W["moe_router"], W)
            elif st == "mix1":
                stage_mix1(C, xs[i], xs[i + 1], W["mem"], W)
            elif st == "mix0":
                stage_mix0(C, xs[i], xs[i + 1], W["mem"], W)
            C.S.barrier()
        C.S.emit()
    return nc


def stage_host_inputs(stages, inputs, consts):
    m = {"c_ident": consts["c_ident"]}
    for st in stages:
        if st == "ffn0":
            m["norm_ffn0"] = pc(inputs["norm_ffn"][0])
            m["ffn_w_gate"] = tile_gu(inputs["ffn_w_gate"])
            m["ffn_w_up"] = tile_gu(inputs["ffn_w_up"])
            m["ffn_w_down"] = tile_d(inputs["ffn_w_down"])
        elif st == "moes":
            m["norm_ffn1"] = pc(inputs["norm_ffn"][1])
            m["norm_ffn1_row"] = np.ascontiguousarray(inputs["norm_ffn"][1], dtype=np.float32)
            m["moe_router"] = np.ascontiguousarray(inputs["moe_router"][0])
            m["moe_w_gate"] = tile_gu(inputs["moe_w_gate"][0])
            m["moe_w_up"] = tile_gu(inputs["moe_w_up"][0])
            m["moe_w_down"] = tile_d(inputs["moe_w_down"][0])
            for k in ("c_iota", "c_slotcol", "c_tri"):
                m[k] = consts[k]
        elif st == "moe":
            m["norm_ffn1"] = pc(inputs["norm_ffn"][1])
            m["moe_router"] = np.ascontiguousarray(inputs["moe_router"][0])
            m["moe_w_gate"] = tile_gu(inputs["moe_w_gate"][0])
            m["moe_w_up"] = tile_gu(inputs["moe_w_up"][0])
            m["moe_w_down"] = tile_d(inputs["moe_w_down"][0])
            m["c_sel"] = consts["c_sel"]
        elif st == "mix1":
            m["norm_mix1"] = pc(inputs["norm_mix"][1]); m["norm_mem1"] = pc(inputs["norm_mem"][1])
            m["w_mem_kv1"] = np.ascontiguousarray(inputs["w_mem_kv"][1]); m["w_out1"] = np.ascontiguousarray(inputs["w_out"][1])
            m["xq_norm1"] = pc(inputs["xq_norm"][1]); m["xk_norm1"] = pc(inputs["xk_norm"][1])
            m["at_w_in"] = np.ascontiguousarray(inputs["at_w_in"][0])
            m["at_q_norm"] = pc(inputs["at_q_norm"][0]); m["at_k_norm"] = pc(inputs["at_k_norm"][0])
            m["rel_bias"] = np.ascontiguousarray(inputs["rel_bias"], dtype=np.float32)
            m["c_oh"] = consts["c_oh"]; m["c_negmask"] = consts["c_negmask"]
        elif st == "mix0":
            m["norm_mix0"] = pc(inputs["norm_mix"][0]); m["norm_mem0"] = pc(inputs["norm_mem"][0])
            m["w_mem_kv0"] = np.ascontiguousarray(inputs["w_mem_kv"][0]); m["w_out0"] = np.ascontiguousarray(inputs["w_out"][0])
            m["xq_norm0"] = pc(inputs["xq_norm"][0]); m["xk_norm0"] = pc(inputs["xk_norm"][0])
            m["hy_w_in"] = np.ascontiguousarray(inputs["hy_w_in"][0])
            cw = np.asarray(inputs["hy_conv_w"][0], np.float32); cb = np.asarray(inputs["hy_conv_b"][0], np.float32)
            cwb = np.concatenate([cw, cb[None, :]], axis=0)
            m["hy_cwb"] = np.ascontiguousarray(cwb.reshape(4, 12, 128).transpose(2, 1, 0))
            m["hy_fsm"] = np.ascontiguousarray(np.stack([inputs["hy_sin_freq"][0], inputs["hy_filt_b1"][0], inputs["hy_filt_b2"][0]], axis=1), dtype=np.float32)
            m["hy_filt_w1"] = np.ascontiguousarray(inputs["hy_filt_w1"][0]); m["hy_filt_w2"] = np.ascontiguousarray(inputs["hy_filt_w2"][0])
            m["hy_filt_w3"] = np.ascontiguousarray(inputs["hy_filt_w3"][0]); m["hy_skip"] = np.ascontiguousarray(inputs["hy_skip"][0])
            for k in ("c_featsT", "c_decay", "c_dftc", "c_dfts", "c_altcol", "c_altrow"):
                m[k] = consts[k]
    return m


def run_stages(stages, x_list, inputs, core_ids=None, mem_list=None):
    consts = host_consts()
    nc = build(stages)
    wm = stage_host_inputs(stages, inputs, consts)
    n = len(x_list)
    in_maps = []
    for i in range(n):
        d = dict(wm)
        d["x_in"] = np.ascontiguousarray(x_list[i], dtype=np.float32)
        if mem_list is not None and any(st in ("mix0", "mix1") for st in stages):
            d["mem"] = np.ascontiguousarray(mem_list[i], dtype=np.float32)
        in_maps.append(d)
    res = run_bass_kernel_spmd(nc, in_maps, core_ids=list(range(n)) if core_ids is None else core_ids)
    return [r["x_out"] for r in res.results]


ALL_STAGES = ["mix0", "ffn0", "mix1", "moes"]


def kernel(**inputs):
    inputs = {k: np.asarray(v) for k, v in inputs.items()}
    B = inputs["x"].shape[0]
    outs = run_stages(ALL_STAGES, [inputs["x"][b] for b in range(B)], inputs,
                      mem_list=[inputs["mem"][b] for b in range(B)])
    return np.stack(outs, axis=0).astype(np.float32)
```
